# Optimizing a Trainium2 kernel written in Bass

```python
import jax, jax.numpy as jnp
from jax import lax
import numpy as np

D_MODEL = 1024
BATCH = 8
SEQ = 4096
DEPTH = 2

D_CONV = D_MODEL // 2
D_POOL = D_MODEL // 2
CONV_K = 31
POOL_WINDOWS = (2, 4, 8, 16)
N_POOL_GROUPS = len(POOL_WINDOWS)
POOL_GROUP = D_POOL // N_POOL_GROUPS
POOL_OUT_GROUP = D_MODEL // N_POOL_GROUPS
N_BRANCHES = 2
D_IN = 2 * D_CONV + D_POOL + N_BRANCHES * D_MODEL

N_EXPERT_GROUPS = 4
EXPERTS_PER_GROUP = 8
N_EXPERTS = N_EXPERT_GROUPS * EXPERTS_PER_GROUP
TOP_K = 2
D_EXPERT = D_MODEL // 4

N_MOD = 6
EPS = 1e-6

kernel_name = "hybrid_conformer_pool_hmoe_adaln"


def rms_norm(x, g):
    xf = x.astype(jnp.float32)
    y = xf * lax.rsqrt(jnp.mean(xf * xf, axis=-1, keepdims=True) + EPS)
    return (y * g.astype(jnp.float32)).astype(x.dtype)


def layer_norm(x, g, b):
    xf = x.astype(jnp.float32)
    mu = jnp.mean(xf, axis=-1, keepdims=True)
    var = jnp.mean(jnp.square(xf - mu), axis=-1, keepdims=True)
    y = (xf - mu) * lax.rsqrt(var + EPS)
    return (y * g.astype(jnp.float32) + b.astype(jnp.float32)).astype(x.dtype)


def modulate(h, shift, scale):
    return h * (1 + scale[:, None, :]) + shift[:, None, :]


def conformer_conv_branch(u, conv_w, conv_b, ln_g, ln_b, w_pw, b_pw):
    a, g = jnp.split(u, 2, axis=-1)
    v = a * jax.nn.sigmoid(g)
    v = lax.conv_general_dilated(
        v, conv_w[:, None, :], window_strides=(1,), padding=[(CONV_K - 1, 0)],
        dimension_numbers=("NWC", "WIO", "NWC"), feature_group_count=D_CONV) + conv_b
    v = jax.nn.silu(layer_norm(v, ln_g, ln_b))
    return v @ w_pw + b_pw


def causal_window_mean(u, w):
    s_len = u.shape[1]
    cs = jnp.cumsum(u.astype(jnp.float32), axis=1)
    shifted = jnp.pad(cs, ((0, 0), (w, 0), (0, 0)))[:, :s_len]
    count = jnp.minimum(jnp.arange(1, s_len + 1), w).astype(jnp.float32)
    return ((cs - shifted) / count[None, :, None]).astype(u.dtype)


def pool_branch(u, pool_w, pool_scale):
    b, s, _ = u.shape
    ug = u.reshape(b, s, N_POOL_GROUPS, POOL_GROUP)
    pooled = jnp.stack([causal_window_mean(ug[:, :, gi], w)
                        for gi, w in enumerate(POOL_WINDOWS)], axis=2) - ug
    y = jnp.einsum("bsgc,gco->bsgo", pooled, pool_w).reshape(b, s, D_MODEL)
    return y * pool_scale


def hierarchical_moe(h, w_rg, b_rg, w_re, b_re, w_g, w_u, w_d):
    b, s, d = h.shape
    t = h.reshape(b * s, d)
    n_tok = t.shape[0]
    group_probs = jax.nn.softmax((t @ w_rg + b_rg).astype(jnp.float32), axis=-1)
    p_group, g_idx = lax.top_k(group_probs, 1)
    exp_logits = (t @ w_re + b_re).astype(jnp.float32).reshape(n_tok, N_EXPERT_GROUPS, EXPERTS_PER_GROUP)
    in_group = jnp.take_along_axis(exp_logits, g_idx[:, :, None], axis=1)[:, 0]
    p_exp, e_idx = lax.top_k(jax.nn.softmax(in_group, axis=-1), TOP_K)
    weights = p_group * p_exp / jnp.sum(p_exp, axis=-1, keepdims=True)
    expert_id = g_idx * EXPERTS_PER_GROUP + e_idx
    combine = jnp.sum(jax.nn.one_hot(expert_id, N_EXPERTS, dtype=jnp.float32) * weights[..., None],
                      axis=1).astype(h.dtype)
    out = jnp.zeros_like(t)
    for gi in range(N_EXPERT_GROUPS):
        sl = slice(gi * EXPERTS_PER_GROUP, (gi + 1) * EXPERTS_PER_GROUP)
        act = jax.nn.silu(jnp.einsum("td,edh->teh", t, w_g[sl])) * jnp.einsum("td,edh->teh", t, w_u[sl])
        out = out + jnp.einsum("teh,ehd->td", act * combine[:, sl, None], w_d[sl])
    return out.reshape(b, s, d)


def setup_inputs(seed: int = 0) -> dict:
    key = jax.random.key(seed)
    ks = jax.random.split(key, 24)
    f32 = jnp.float32
    nrm = lambda k, shape, scale: (jax.random.normal(k, shape, f32) * scale)
    return {
        "x": nrm(ks[0], (BATCH, SEQ, D_MODEL), 1.0),
        "c": nrm(ks[1], (BATCH, D_MODEL), 1.0),
        "mixer_norm_g": 1.0 + nrm(ks[2], (DEPTH, D_MODEL), 0.05),
        "w_ada": nrm(ks[3], (DEPTH, D_MODEL, N_MOD * D_MODEL), 0.5 * D_MODEL ** -0.5),
        "b_ada": nrm(ks[4], (DEPTH, N_MOD * D_MODEL), 0.02),
        "w_in": nrm(ks[5], (DEPTH, D_MODEL, D_IN), D_MODEL ** -0.5),
        "conv_w": nrm(ks[6], (DEPTH, CONV_K, D_CONV), CONV_K ** -0.5),
        "conv_b": nrm(ks[7], (DEPTH, D_CONV), 0.02),
        "conv_ln_g": 1.0 + nrm(ks[8], (DEPTH, D_CONV), 0.05),
        "conv_ln_b": nrm(ks[9], (DEPTH, D_CONV), 0.02),
        "w_conv_out": nrm(ks[10], (DEPTH, D_CONV, D_MODEL), D_CONV ** -0.5),
        "b_conv_out": nrm(ks[11], (DEPTH, D_MODEL), 0.02),
        "pool_w": nrm(ks[12], (DEPTH, N_POOL_GROUPS, POOL_GROUP, POOL_OUT_GROUP), POOL_GROUP ** -0.5),
        "pool_scale": 1.0 + nrm(ks[13], (DEPTH, D_MODEL), 0.1),
        "w_out": nrm(ks[14], (DEPTH, D_MODEL, D_MODEL), D_MODEL ** -0.5),
        "ffn_norm_g": 1.0 + nrm(ks[15], (DEPTH, D_MODEL), 0.05),
        "w_router_group": nrm(ks[16], (DEPTH, D_MODEL, N_EXPERT_GROUPS), D_MODEL ** -0.5),
        "b_router_group": nrm(ks[17], (DEPTH, N_EXPERT_GROUPS), 0.01),
        "w_router_expert": nrm(ks[18], (DEPTH, D_MODEL, N_EXPERTS), D_MODEL ** -0.5),
        "b_router_expert": nrm(ks[19], (DEPTH, N_EXPERTS), 0.01),
        "w_expert_gate": nrm(ks[20], (DEPTH, N_EXPERTS, D_MODEL, D_EXPERT), D_MODEL ** -0.5),
        "w_expert_up": nrm(ks[21], (DEPTH, N_EXPERTS, D_MODEL, D_EXPERT), D_MODEL ** -0.5),
        "w_expert_down": nrm(ks[22], (DEPTH, N_EXPERTS, D_EXPERT, D_MODEL), D_EXPERT ** -0.5),
        "final_norm_g": 1.0 + nrm(ks[23], (D_MODEL,), 0.05),
    }


def reference(x, c, mixer_norm_g, w_ada, b_ada, w_in, conv_w, conv_b, conv_ln_g, conv_ln_b,
              w_conv_out, b_conv_out, pool_w, pool_scale, w_out, ffn_norm_g,
              w_router_group, b_router_group, w_router_expert, b_router_expert,
              w_expert_gate, w_expert_up, w_expert_down, final_norm_g):
    c_act = jax.nn.silu(c)
    for l in range(DEPTH):
        mod = c_act @ w_ada[l] + b_ada[l]
        sh1, sc1, g1, sh2, sc2, g2 = jnp.split(mod, N_MOD, axis=-1)
        h = modulate(rms_norm(x, mixer_norm_g[l]), sh1, sc1)
        proj = h @ w_in[l]
        u_conv = proj[..., :2 * D_CONV]
        u_pool = proj[..., 2 * D_CONV:2 * D_CONV + D_POOL]
        gate_a, gate_b = jnp.split(jax.nn.sigmoid(proj[..., 2 * D_CONV + D_POOL:]), N_BRANCHES, axis=-1)
        y_a = conformer_conv_branch(u_conv, conv_w[l], conv_b[l], conv_ln_g[l], conv_ln_b[l],
                                    w_conv_out[l], b_conv_out[l])
        y_b = pool_branch(u_pool, pool_w[l], pool_scale[l])
        mixed = gate_a * y_a + gate_b * y_b
        x = x + g1[:, None, :] * (mixed @ w_out[l])
        h2 = modulate(rms_norm(x, ffn_norm_g[l]), sh2, sc2)
        x = x + g2[:, None, :] * hierarchical_moe(h2, w_router_group[l], b_router_group[l],
                                                  w_router_expert[l], b_router_expert[l],
                                                  w_expert_gate[l], w_expert_up[l], w_expert_down[l])
    return rms_norm(x, final_norm_g)
```

```python
import contextlib
import numpy as np
import concourse.bass as bass
import concourse.mybir as mybir
from concourse.bass_utils import run_bass_kernel_spmd

F32 = mybir.dt.float32
BF16 = mybir.dt.bfloat16
AF = mybir.ActivationFunctionType
ALU = mybir.AluOpType
AX = mybir.AxisListType

D = 1024
NCH = 8
TT = 512
CONV_K = 31
HV = CONV_K - 1
HU = 15
POOL_W = (2, 4, 8, 16)
NE = 32
EPS = 1e-6

C_IDENT = 0
C_C = 128
C_FNG = 136
C_INVC = 144
C_L0 = 160
PL = 216
O_MNG, O_BADA, O_CONVW, O_CONVB, O_LNG, O_LNB, O_BCO, O_PSC, O_FNG2 = 0, 8, 56, 180, 184, 188, 192, 200, 208
NCOL = C_L0 + 2 * PL
NROW = 2 * 36

ENGS = ("pe", "act", "dve", "pool", "sp")


class Sched:
    def __init__(self, nc):
        self.nc = nc
        self.ops = {e: [] for e in ENGS}
        self.semval = {}
        self.seen = {e: {} for e in ENGS}
        self.buf = {}
        self.own = {e: "c_" + e for e in ENGS}
        for e in ENGS:
            self.semval[self.own[e]] = 0

    def _b(self, k):
        if k not in self.buf:
            self.buf[k] = [None, {}]
        return self.buf[k]

    def op(self, eng, fn, reads=(), writes=(), sem=None, inc=1):
        own = self.own[eng]
        deps = {}

        def add(s, v, kind):
            if s == own and sem is None and eng == "pe":
                return
            if deps.get(s, 0) < v:
                deps[s] = v

        for b in reads:
            w = self._b(b)[0]
            if w:
                add(w[0], w[1], "raw")
            if b.startswith("ps"):
                for s, v in self._b(b)[1].items():
                    if s != own:
                        add(s, v, "psrd")
        for b in writes:
            st = self._b(b)
            if st[0]:
                add(st[0][0], st[0][1], "waw")
            for s, v in st[1].items():
                add(s, v, "war")
        waits = []
        for s, v in deps.items():
            if self.seen[eng].get(s, 0) < v:
                self.seen[eng][s] = v
                waits.append((s, v))
        semname = sem or own
        if semname not in self.semval:
            self.semval[semname] = 0
        if inc:
            self.semval[semname] += inc
            val = self.semval[semname]
        else:
            val = self.semval[semname] + 1
        self.ops[eng].append((waits, fn, semname, inc))
        for b in reads:
            st = self._b(b)
            if st[1].get(semname, 0) < val:
                st[1][semname] = val
        for b in writes:
            st = self._b(b)
            st[0] = (semname, val)
            st[1] = {}
        return val

    def dma(self, eng, out, in_, sem, reads=(), writes=()):
        return self.op(eng, lambda e: e.dma_start(out=out, in_=in_), reads=reads, writes=writes, sem=sem, inc=16)

    def final_wait(self, eng, sems):
        waits = [(s, self.semval[s]) for s in sems if self.semval.get(s, 0) > 0]
        self.ops[eng].append((waits, None, None, 0))

    def emit(self):
        nc = self.nc
        with contextlib.ExitStack() as st:
            handles = {}
            for name in self.semval:
                handles[name] = st.enter_context(nc.semaphore(name))
            block = st.enter_context(nc.Block())

            def run(engobj, lst):
                for waits, fn, semname, inc in lst:
                    for s, v in waits:
                        engobj.wait_ge(handles[s], v)
                    if fn is None:
                        continue
                    ins = fn(engobj)
                    if inc:
                        ins.then_inc(handles[semname], inc)

            @block.tensor
            def _(e):
                run(e, self.ops["pe"])

            @block.scalar
            def _(e):
                run(e, self.ops["act"])

            @block.vector
            def _(e):
                run(e, self.ops["dve"])

            @block.gpsimd
            def _(e):
                run(e, self.ops["pool"])

            @block.sync
            def _(e):
                run(e, self.ops["sp"])


def build_nc(S=4096, NL=2, final_norm=True, first_layer=0):
    NT = S // TT
    nc = bass.Bass("TRN2", target_bir_lowering=False)

    def din(name, shape):
        return nc.dram_tensor(name, list(shape), F32, kind="ExternalInput").ap()

    xT = din("xT", [D, S])
    colp_d = din("colp", [128, NCOL])
    rowp_d = din("rowp", [128, NROW])
    w_ada = din("w_ada", [2, D, 6 * D])
    w_in = din("w_in", [2, D, 3584])
    w_co = din("w_co", [2, 512, D])
    w_pool = din("w_pool", [2, 512, 256])
    w_out = din("w_out", [2, D, D])
    w_r = din("w_r", [2, D, 36])
    w_g = din("w_g", [2, NE, D, 256])
    w_u = din("w_u", [2, NE, D, 256])
    w_d = din("w_d", [2, NE, 256, D])
    yT = nc.dram_tensor("yT", [D, S], F32, kind="ExternalOutput").ap()
    combT_d = nc.dram_tensor("combT_d", [32, TT], BF16, kind="Internal").ap()

    S_ = Sched(nc)
    st = contextlib.ExitStack()
    with st:
        def sb(name, shape, dt):
            return st.enter_context(nc.sbuf_tensor(name, list(shape), dt))

        colp = sb("colp_s", [128, NCOL], F32)
        rowp = sb("rowp_s", [128, NROW], F32)
        x = sb("x", [128, NCH, TT], F32)
        h = sb("h", [128, NCH, TT], BF16)
        mix = sb("mix", [128, NCH, TT], BF16)
        NTMP = 4
        tmp = [sb(f"tmp{i}", [128, TT], F32) for i in range(NTMP)]
        rstd = sb("rstd", [128, TT], F32)
        stat = [sb(f"stat{i}", [128, TT], F32) for i in range(3)]
        vb = sb("vb", [128, 4, HV + TT], BF16)
        vhalo = sb("vhalo", [128, 2, 4, HV], BF16)
        U = sb("U", [128, 4, HU + TT], F32)
        uhalo = sb("uhalo", [128, 2, 4, HU], F32)
        T1 = sb("T1", [128, HU + TT], F32)
        T2 = sb("T2", [128, HU + TT], F32)
        pooled = sb("pooled", [128, 4, TT], BF16)
        cvo = sb("cvo", [128, 4, TT], F32)
        cv = sb("cv", [128, 4, TT], BF16)
        NDG = 2
        dg = [sb(f"dg{i}", [128, CONV_K, 128], BF16) for i in range(NDG)]
        NWS = 4
        wsl = [sb(f"wsl{i}", [128, NCH, 512], BF16) for i in range(NWS)]
        wco = sb("wco", [128, 4, D], BF16)
        pw = sb("pw", [128, 4, 256], BF16)
        wr = sb("wr", [128, 2, NCH, 36], BF16)
        NGU = 3
        NED = 4
        egu = [sb(f"egu{i}", [128, NCH, 512], BF16) for i in range(NGU)]
        ed = [sb(f"ed{i}", [128, 2, D], BF16) for i in range(NED)]
        sgt = [sb(f"sgt{i}", [128, 2, TT], BF16) for i in range(2)]
        tbt = [sb(f"tbt{i}", [128, 2, TT], BF16) for i in range(2)]
        NACT = 4
        actt = [sb(f"actt{i}", [128, 2, TT], BF16) for i in range(NACT)]
        NCB = 3
        cbc = [sb(f"cbc{i}", [128, TT], BF16) for i in range(NCB)]
        combT = sb("combT", [32, TT], BF16)
        ident_bf = sb("ident_bf", [128, 128], BF16)
        ones_bf = sb("ones_bf", [128, 128], BF16)
        epsT = sb("epsT", [128, 1], F32)
        cact = sb("cact", [128, NCH], BF16)
        mod = sb("mod", [128, 2, 48], F32)
        gs1 = sb("gs1", [128, 2, NCH], F32)
        gs2 = sb("gs2", [128, 2, NCH], F32)
        rl = sb("rl", [128, 36], F32)
        r4 = [sb(f"r4_{i}", [128, 4], F32) for i in range(4)]
        r8 = [sb(f"r8_{i}", [128, 8], F32) for i in range(5)]
        r1 = [sb(f"r1_{i}", [128, 1], F32) for i in range(8)]
        comb = sb("comb", [128, NE], F32)
        ps = [st.enter_context(nc.psum_tensor(f"ps{i}", [128, TT], F32)) for i in range(8)]

        ident = colp[:, C_IDENT:C_IDENT + 128]

        bank_ctr = [0]

        def bank():
            b = bank_ctr[0] % 8
            bank_ctr[0] += 1
            return b

        tmp_ctr = [0]

        def newtmp():
            i = tmp_ctr[0] % NTMP
            tmp_ctr[0] += 1
            return i

        def lcol(l, off, n=1):
            c = C_L0 + l * PL + off
            return colp[:, c:c + n]

        def mm_group(b, pairs, reads, M=128, N=TT, out=None):
            n = len(pairs)
            o = out if out is not None else ps[b][0:M, 0:N]
            for i, (lt, rh) in enumerate(pairs):
                S_.op("pe", (lambda e, lt=lt, rh=rh, i=i: e.matmul(o, lhsT=lt, rhs=rh, start=(i == 0), stop=(i == n - 1))),
                      reads=reads, writes=[f"ps{b}"], inc=1 if i == n - 1 else 0)

        wblocks = []
        for t_ in range(NT):
            for l_ in range(first_layer, first_layer + NL):
                wl_ = w_in[l_].rearrange("(kc p) n -> p kc n", p=128)
                wo_ = w_out[l_].rearrange("(kc p) n -> p kc n", p=128)
                for c0 in (0, 512, 1024, 1536, 2560, 2048, 3072):
                    wblocks.append(wl_[:, :, c0:c0 + 512])
                wblocks.append(wo_[:, :, 0:512])
                wblocks.append(wo_[:, :, 512:1024])
        ws_loaded = [0]

        def prefetch_to(k):
            while ws_loaded[0] < min(k, len(wblocks)):
                n = ws_loaded[0]
                i = n % NWS
                S_.dma("pool", wsl[i][:], wblocks[n], f"s_ws{i}", writes=[f"ws{i}"])
                ws_loaded[0] += 1

        mix_it = [0]

        S_.dma("sp", colp[:], colp_d, "s_colp", writes=["colp"])
        S_.dma("sp", rowp[:], rowp_d, "s_rowp", writes=["rowp"])
        S_.dma("pool", wr[:], w_r.rearrange("l (kc p) n -> p l kc n", p=128), "s_wr", writes=["wr"])
        S_.op("dve", lambda e: e.memset(ones_bf[:], 1.0), writes=["ones"])
        S_.op("dve", lambda e: e.memset(epsT[:], EPS), writes=["eps"])
        S_.op("dve", lambda e: e.tensor_copy(out=ident_bf[:], in_=ident), reads=["colp"], writes=["identbf"])
        S_.op("dve", lambda e: e.memset(vhalo[:], 0.0), writes=["vhalo"])
        S_.op("dve", lambda e: e.memset(uhalo[:], 0.0), writes=["uhalo"])
        S_.op("act", lambda e: e.activation(out=cact[:], in_=colp[:, C_C:C_C + NCH], func=AF.Silu), reads=["colp"], writes=["cact"])

        for l in range(first_layer, first_layer + NL):
            bm = bank()
            for j in range(24):
                sl = j % (2 * NGU)
                stage = egu[sl // 2][:, :, (sl % 2) * 256:(sl % 2 + 1) * 256]
                key = ("eg%d" if sl % 2 == 0 else "eu%d") % (sl // 2)
                S_.dma("pool", stage, w_ada[l, :, j * 256:(j + 1) * 256].rearrange("(kc p) n -> p kc n", p=128),
                       "s_" + key, writes=[key])
                for half in range(2):
                    cj = 2 * j + half
                    for kc in range(NCH):
                        S_.op("pe", (lambda e, stage=stage, half=half, kc=kc, cj=cj, bm=bm:
                                     e.matmul(ps[bm][:, cj:cj + 1], lhsT=stage[:, kc, half * 128:(half + 1) * 128],
                                              rhs=cact[:, kc:kc + 1], start=(kc == 0), stop=(kc == NCH - 1))),
                              reads=[key, "cact"], writes=[f"ps{bm}"], inc=1 if kc == NCH - 1 else 0)
            S_.op("dve", (lambda e, l=l, bm=bm: e.tensor_tensor(out=mod[:, l, :], in0=ps[bm][:, 0:48], in1=lcol(l, O_BADA, 48), op=ALU.add)),
                  reads=[f"ps{bm}", "colp"], writes=["mod"])
            S_.op("dve", (lambda e, l=l: e.scalar_tensor_tensor(out=gs1[:, l, :], in0=mod[:, l, 8:16], scalar=1.0, in1=lcol(l, O_MNG, 8),
                                                                 op0=ALU.add, op1=ALU.mult)),
                  reads=["mod", "colp"], writes=["gs1"])
            S_.op("dve", (lambda e, l=l: e.scalar_tensor_tensor(out=gs2[:, l, :], in0=mod[:, l, 32:40], scalar=1.0, in1=lcol(l, O_FNG2, 8),
                                                                 op0=ALU.add, op1=ALU.mult)),
                  reads=["mod", "colp"], writes=["gs2"])

        XK = [f"x{dc}" for dc in range(NCH)]
        HK = [f"h{dc}" for dc in range(NCH)]
        MK = [f"mix{dc}" for dc in range(NCH)]

        def rms_stats():
            for dc in range(NCH):
                S_.op("act", (lambda e, dc=dc: e.activation(out=mix[:, dc, :], in_=x[:, dc, :], func=AF.Square)),
                      reads=[XK[dc]], writes=[MK[dc]])
            b = bank()
            mm_group(b, [(ones_bf[:], mix[:, dc, :]) for dc in range(NCH)], reads=["ones"] + MK)
            S_.op("act", (lambda e, b=b: e.activation(out=rstd[:], in_=ps[b][:], func=AF.Sqrt, bias=epsT[:, 0:1], scale=1.0 / D)),
                  reads=[f"ps{b}", "eps"], writes=["rstd"])
            S_.op("dve", lambda e: e.reciprocal(out=rstd[:], in_=rstd[:]), reads=["rstd"], writes=["rstd"])

        def norm_to_h(gs_ap, sh_ap):
            rms_stats()
            for dc in range(NCH):
                ti = newtmp()
                S_.op("dve", (lambda e, dc=dc, ti=ti: e.tensor_tensor(out=tmp[ti][:], in0=x[:, dc, :], in1=rstd[:], op=ALU.mult)),
                      reads=[XK[dc], "rstd"], writes=[f"tmp{ti}"])
                S_.op("act", (lambda e, dc=dc, ti=ti: e.activation(out=h[:, dc, :], in_=tmp[ti][:], func=AF.Identity,
                                                                   bias=sh_ap[:, dc:dc + 1], scale=gs_ap[:, dc:dc + 1])),
                      reads=[f"tmp{ti}", "mod", "gs1", "gs2"], writes=[HK[dc]])

        dg_ctr = [0]

        def mixer(t, l):
            first = (t == 0)
            norm_to_h(gs1[:, l, :], mod[:, l, 0:8])
            base = 9 * mix_it[0]
            mix_it[0] += 1
            blk = lambda n: (base + n) % NWS
            import os as _os2
            _stop = int(_os2.environ.get("K_MIX_STOP", "9"))
            if _stop <= 1:
                return
            prefetch_to(base + 0 + NWS)
            S_.dma("pool", wco[:], w_co[l].rearrange("(cc p) n -> p cc n", p=128), "s_wco", writes=["wco"])
            S_.dma("pool", pw[:], w_pool[l].rearrange("(g p) n -> p g n", p=128), "s_pw", writes=["pw"])
            iA, iG, iU = blk(0), blk(1), blk(2)
            S_.op("pool", (lambda e: e.tensor_copy(out=vb[:, :, 0:HV], in_=vhalo[:, l, :, :])), reads=["vhalo"], writes=["vb"])
            for cc in range(4):
                ba = bank()
                mm_group(ba, [(wsl[iA][:, kc, cc * 128:(cc + 1) * 128], h[:, kc, :]) for kc in range(NCH)], reads=[f"ws{iA}"] + HK)
                bg = bank()
                mm_group(bg, [(wsl[iG][:, kc, cc * 128:(cc + 1) * 128], h[:, kc, :]) for kc in range(NCH)], reads=[f"ws{iG}"] + HK)
                ti = newtmp()
                S_.op("act", (lambda e, bg=bg, ti=ti: e.activation(out=tmp[ti][:], in_=ps[bg][:], func=AF.Sigmoid)),
                      reads=[f"ps{bg}"], writes=[f"tmp{ti}"])
                S_.op("dve", (lambda e, ba=ba, ti=ti, cc=cc: e.tensor_tensor(out=vb[:, cc, HV:HV + TT], in0=ps[ba][:], in1=tmp[ti][:], op=ALU.mult)),
                      reads=[f"ps{ba}", f"tmp{ti}"], writes=["vb"])
            S_.op("pool", (lambda e: e.tensor_copy(out=vhalo[:, l, :, :], in_=vb[:, :, TT:TT + HV])), reads=["vb"], writes=["vhalo"])
            if _stop <= 2:
                return
            prefetch_to(base + 2 + NWS)
            S_.op("pool", (lambda e: e.tensor_copy(out=U[:, :, 0:HU], in_=uhalo[:, l, :, :])), reads=["uhalo"], writes=["U"])
            for gi in range(4):
                b = bank()
                mm_group(b, [(wsl[iU][:, kc, gi * 128:(gi + 1) * 128], h[:, kc, :]) for kc in range(NCH)], reads=[f"ws{iU}"] + HK)
                S_.op("act", (lambda e, b=b, gi=gi: e.activation(out=U[:, gi, HU:HU + TT], in_=ps[b][:], func=AF.Copy)),
                      reads=[f"ps{b}"], writes=["U"])
            S_.op("pool", (lambda e: e.tensor_copy(out=uhalo[:, l, :, :], in_=U[:, :, TT:TT + HU])), reads=["U"], writes=["uhalo"])
            W_ = HU + TT
            for gi, w in enumerate(POOL_W):
                src, srck = U[:, gi, :], "U"
                lo = 0
                dsts = [(T1, "T1"), (T2, "T2")]
                for j in range(gi + 1):
                    sh = 1 << j
                    dst, dstk = dsts[j % 2]
                    S_.op("pool", (lambda e, dst=dst, src=src, lo=lo, sh=sh: e.tensor_tensor(
                        out=dst[:, lo + sh:W_], in0=src[:, lo + sh:W_], in1=src[:, lo:W_ - sh], op=ALU.add)),
                        reads=[srck], writes=[dstk])
                    src, srck = dst[:], dstk
                    lo += sh
                fxb, fxk = dsts[(gi + 1) % 2]
                S_.op("pool", (lambda e, src=src, fxb=fxb, w=w: e.tensor_scalar(
                    out=fxb[:, HU:W_], in0=src[:, HU:W_], scalar1=1.0 / w, scalar2=None, op0=ALU.mult)),
                    reads=[srck], writes=[fxk])
                S_.op("pool", (lambda e, fxb=fxb, gi=gi: e.tensor_tensor(
                    out=pooled[:, gi, :], in0=fxb[:, HU:W_], in1=U[:, gi, HU:W_], op=ALU.subtract)),
                    reads=[fxk, "U"], writes=["pooled"])
                if first:
                    n = w - 1
                    fxb, fxk = dsts[(gi + 1) % 2]
                    S_.op("pool", (lambda e, src=src, n=n, fxb=fxb: e.tensor_tensor(out=fxb[:, 0:n], in0=src[:, HU:HU + n],
                                                                                   in1=colp[:, C_INVC:C_INVC + n], op=ALU.mult)),
                          reads=[srck, "colp"], writes=[fxk])
                    S_.op("pool", (lambda e, fxb=fxb, n=n, gi=gi: e.tensor_tensor(out=pooled[:, gi, 0:n], in0=fxb[:, 0:n], in1=U[:, gi, HU:HU + n], op=ALU.subtract)),
                          reads=[fxk, "U"], writes=["pooled"])
            if _stop <= 3:
                return
            bs1 = bank()
            bs2 = bank()
            for cc in range(4):
                di = dg_ctr[0] % NDG
                dg_ctr[0] += 1
                _skip = _os2.environ.get("K_CONV_SKIP", "")
                for k in range(CONV_K if "g" not in _skip else 0):
                    S_.op(_os2.environ.get("K_DG_ENG", "dve"), (lambda e, di=di, k=k, cc=cc: e.tensor_scalar(out=dg[di][:, k, :], in0=ident_bf[:], scalar1=lcol(l, O_CONVW + k * 4 + cc),
                                                                               scalar2=None, op0=ALU.mult)),
                          reads=["identbf", "colp"], writes=[f"dg{di}"])
                b = bank()
                _nk = int(_os2.environ.get("K_CONV_NK", str(CONV_K)))
                if "m" not in _skip:
                    mm_group(b, [(dg[di][:, k, :], vb[:, cc, k:k + TT]) for k in range(_nk)], reads=[f"dg{di}", "vb"])
                if "e" in _skip:
                    continue
                S_.op("act", (lambda e, b=b, cc=cc: e.activation(out=cvo[:, cc, :], in_=ps[b][:], func=AF.Identity, bias=lcol(l, O_CONVB + cc), scale=1.0)),
                      reads=[f"ps{b}", "colp"], writes=[f"cvo{cc}"])
                S_.op("dve", (lambda e, cc=cc: e.tensor_copy(out=mix[:, cc, :], in_=cvo[:, cc, :])), reads=[f"cvo{cc}"], writes=[MK[cc]])
                S_.op("act", (lambda e, cc=cc: e.activation(out=mix[:, 4 + cc, :], in_=cvo[:, cc, :], func=AF.Square)),
                      reads=[f"cvo{cc}"], writes=[MK[4 + cc]])
            _sub = int(_os2.environ.get("K_CONV_SUB", "9"))
            if _sub <= 1:
                return
            mm_group(bs1, [(ones_bf[:], mix[:, cc, :]) for cc in range(4)], reads=["ones"] + MK[0:4])
            mm_group(bs2, [(ones_bf[:], mix[:, 4 + cc, :]) for cc in range(4)], reads=["ones"] + MK[4:8])
            S_.op("dve", (lambda e: e.tensor_scalar(out=stat[0][:], in0=ps[bs1][:], scalar1=1.0 / 512, scalar2=None, op0=ALU.mult)),
                  reads=[f"ps{bs1}"], writes=["stat0"])
            S_.op("dve", (lambda e: e.tensor_tensor(out=stat[1][:], in0=stat[0][:], in1=stat[0][:], op=ALU.mult)), reads=["stat0"], writes=["stat1"])
            S_.op("dve", (lambda e: e.scalar_tensor_tensor(out=stat[1][:], in0=ps[bs2][:], scalar=1.0 / 512, in1=stat[1][:], op0=ALU.mult, op1=ALU.subtract)),
                  reads=[f"ps{bs2}", "stat1"], writes=["stat1"])
            S_.op("act", (lambda e: e.activation(out=stat[2][:], in_=stat[1][:], func=AF.Sqrt, bias=epsT[:, 0:1], scale=1.0)),
                  reads=["stat1", "eps"], writes=["stat2"])
            S_.op("dve", (lambda e: e.reciprocal(out=stat[2][:], in_=stat[2][:])), reads=["stat2"], writes=["stat2"])
            if _sub <= 2:
                return
            for cc in range(4):
                S_.op("dve", (lambda e, cc=cc: e.tensor_tensor(out=cvo[:, cc, :], in0=cvo[:, cc, :], in1=stat[0][:], op=ALU.subtract)),
                      reads=[f"cvo{cc}", "stat0"], writes=[f"cvo{cc}"])
                S_.op("dve", (lambda e, cc=cc: e.tensor_tensor(out=cvo[:, cc, :], in0=cvo[:, cc, :], in1=stat[2][:], op=ALU.mult)),
                      reads=[f"cvo{cc}", "stat2"], writes=[f"cvo{cc}"])
                S_.op("act", (lambda e, cc=cc: e.activation(out=cv[:, cc, :], in_=cvo[:, cc, :], func=AF.Silu, bias=lcol(l, O_LNB + cc), scale=lcol(l, O_LNG + cc))),
                      reads=[f"cvo{cc}", "colp"], writes=[f"cv{cc}"])
            if _stop <= 4:
                return
            CVK = [f"cv{cc}" for cc in range(4)]
            preload_experts(l)
            for half in range(2):
                prefetch_to(base + 3 + 2 * half + NWS)
                iGA, iGB = blk(3 + 2 * half), blk(4 + 2 * half)
                for q in range(4):
                    dc = half * 4 + q
                    bya = bank()
                    mm_group(bya, [(wco[:, cc, dc * 128:(dc + 1) * 128], cv[:, cc, :]) for cc in range(4)], reads=["wco"] + CVK)
                    byb = bank()
                    g = dc // 2
                    mm_group(byb, [(pw[:, g, (dc % 2) * 128:(dc % 2 + 1) * 128], pooled[:, g, :])], reads=["pw", "pooled"])
                    bga = bank()
                    mm_group(bga, [(wsl[iGA][:, kc, q * 128:(q + 1) * 128], h[:, kc, :]) for kc in range(NCH)], reads=[f"ws{iGA}"] + HK)
                    bgb = bank()
                    mm_group(bgb, [(wsl[iGB][:, kc, q * 128:(q + 1) * 128], h[:, kc, :]) for kc in range(NCH)], reads=[f"ws{iGB}"] + HK)
                    ta, tb_ = newtmp(), newtmp()
                    S_.op("act", (lambda e, bga=bga, ta=ta: e.activation(out=tmp[ta][:], in_=ps[bga][:], func=AF.Sigmoid)), reads=[f"ps{bga}"], writes=[f"tmp{ta}"])
                    S_.op("act", (lambda e, bgb=bgb, tb_=tb_: e.activation(out=tmp[tb_][:], in_=ps[bgb][:], func=AF.Sigmoid)), reads=[f"ps{bgb}"], writes=[f"tmp{tb_}"])
                    S_.op("dve", (lambda e, bya=bya, ta=ta, dc=dc: e.scalar_tensor_tensor(out=tmp[ta][:], in0=ps[bya][:], scalar=lcol(l, O_BCO + dc), in1=tmp[ta][:],
                                                                                           op0=ALU.add, op1=ALU.mult)),
                          reads=[f"ps{bya}", f"tmp{ta}", "colp"], writes=[f"tmp{ta}"])
                    S_.op("dve", (lambda e, byb=byb, tb_=tb_, dc=dc: e.scalar_tensor_tensor(out=tmp[tb_][:], in0=ps[byb][:], scalar=lcol(l, O_PSC + dc), in1=tmp[tb_][:],
                                                                                             op0=ALU.mult, op1=ALU.mult)),
                          reads=[f"ps{byb}", f"tmp{tb_}", "colp"], writes=[f"tmp{tb_}"])
                    S_.op("dve", (lambda e, ta=ta, tb_=tb_, dc=dc: e.tensor_tensor(out=mix[:, dc, :], in0=tmp[ta][:], in1=tmp[tb_][:], op=ALU.add)),
                          reads=[f"tmp{ta}", f"tmp{tb_}"], writes=[MK[dc]])
            if _stop <= 5:
                return
            for half in range(2):
                prefetch_to(base + 7 + half + NWS)
                iO = blk(7 + half)
                for q in range(4):
                    dc = half * 4 + q
                    b = bank()
                    mm_group(b, [(wsl[iO][:, kc, q * 128:(q + 1) * 128], mix[:, kc, :]) for kc in range(NCH)], reads=[f"ws{iO}"] + MK)
                    S_.op("dve", (lambda e, b=b, dc=dc: e.scalar_tensor_tensor(out=x[:, dc, :], in0=ps[b][:], scalar=mod[:, l, 16 + dc:17 + dc], in1=x[:, dc, :],
                                                                               op0=ALU.mult, op1=ALU.add)),
                          reads=[f"ps{b}", XK[dc], "mod"], writes=[XK[dc]])

        def load_egu(l, e_):
            i = e_ % NGU
            S_.dma("pool", egu[i][:, :, 0:256], w_g[l, e_].rearrange("(kc p) n -> p kc n", p=128), f"s_eg{i}", writes=[f"eg{i}"])
            S_.dma("pool", egu[i][:, :, 256:512], w_u[l, e_].rearrange("(kc p) n -> p kc n", p=128), f"s_eu{i}", writes=[f"eu{i}"])

        def load_ed(l, e_):
            i = e_ % NED
            S_.dma("pool", ed[i][:], w_d[l, e_].rearrange("(hc p) n -> p hc n", p=128), f"s_ed{i}", writes=[f"ed{i}"])

        def preload_experts(l):
            for e_ in range(NGU):
                load_egu(l, e_)
            for e_ in range(NED):
                load_ed(l, e_)

        def router(l):
            bt = bank()
            for blk in range(4):
                b = bank()
                mm_group(b, [(h[:, kc, blk * 128:(blk + 1) * 128], wr[:, l, kc, :]) for kc in range(NCH)], reads=HK + ["wr"], M=128, N=36)
                V = lambda fn, reads, writes: S_.op("dve", fn, reads=reads, writes=writes)
                V(lambda e, b=b: e.tensor_tensor(out=rl[:], in0=ps[b][:, 0:36], in1=rowp[:, l * 36:(l + 1) * 36], op=ALU.add), [f"ps{b}", "rowp"], ["rl"])
                gmax, ssum, pg, m1, m2, dd, w1, w2 = [r1[i] for i in range(8)]
                gmask, d4, e4, den4 = r4
                sel8, mask1, sel8b, mask2, cw8 = r8
                V(lambda e: e.tensor_reduce(out=gmax[:], in_=rl[:, 0:4], axis=AX.X, op=ALU.max), ["rl"], ["gmax"])
                V(lambda e: e.tensor_scalar(out=gmask[:], in0=rl[:, 0:4], scalar1=gmax[:, 0:1], scalar2=None, op0=ALU.is_equal), ["rl", "gmax"], ["gmask"])
                V(lambda e: e.tensor_scalar(out=d4[:], in0=rl[:, 0:4], scalar1=gmax[:, 0:1], scalar2=None, op0=ALU.subtract), ["rl", "gmax"], ["d4"])
                S_.op("act", lambda e: e.activation(out=d4[:], in_=d4[:], func=AF.Tanh, scale=0.5), reads=["d4"], writes=["d4"])
                V(lambda e: e.tensor_scalar(out=den4[:], in0=d4[:], scalar1=-1.0, scalar2=1.0, op0=ALU.mult, op1=ALU.add), ["d4"], ["den4"])
                V(lambda e: e.reciprocal(out=den4[:], in_=den4[:]), ["den4"], ["den4"])
                V(lambda e: e.scalar_tensor_tensor(out=e4[:], in0=d4[:], scalar=1.0, in1=den4[:], op0=ALU.add, op1=ALU.mult), ["d4", "den4"], ["e4"])
                V(lambda e: e.tensor_reduce(out=ssum[:], in_=e4[:], axis=AX.X, op=ALU.add), ["e4"], ["ssum"])
                V(lambda e: e.reciprocal(out=pg[:], in_=ssum[:]), ["ssum"], ["pg"])
                V(lambda e: e.tensor_scalar(out=sel8[:], in0=rl[:, 4:12], scalar1=gmask[:, 0:1], scalar2=None, op0=ALU.mult), ["rl", "gmask"], ["sel8"])
                for g in range(1, 4):
                    V(lambda e, g=g: e.scalar_tensor_tensor(out=sel8[:], in0=rl[:, 4 + 8 * g:12 + 8 * g], scalar=gmask[:, g:g + 1], in1=sel8[:],
                                                            op0=ALU.mult, op1=ALU.add), ["rl", "gmask", "sel8"], ["sel8"])
                V(lambda e: e.tensor_reduce(out=m1[:], in_=sel8[:], axis=AX.X, op=ALU.max), ["sel8"], ["m1"])
                V(lambda e: e.tensor_scalar(out=mask1[:], in0=sel8[:], scalar1=m1[:, 0:1], scalar2=None, op0=ALU.is_equal), ["sel8", "m1"], ["mask1"])
                V(lambda e: e.scalar_tensor_tensor(out=sel8b[:], in0=mask1[:], scalar=-1e30, in1=sel8[:], op0=ALU.mult, op1=ALU.add), ["mask1", "sel8"], ["sel8b"])
                V(lambda e: e.tensor_reduce(out=m2[:], in_=sel8b[:], axis=AX.X, op=ALU.max), ["sel8b"], ["m2"])
                V(lambda e: e.tensor_scalar(out=mask2[:], in0=sel8b[:], scalar1=m2[:, 0:1], scalar2=None, op0=ALU.is_equal), ["sel8b", "m2"], ["mask2"])
                V(lambda e: e.tensor_tensor(out=dd[:], in0=m1[:], in1=m2[:], op=ALU.subtract), ["m1", "m2"], ["dd"])
                S_.op("act", lambda e: e.activation(out=dd[:], in_=dd[:], func=AF.Tanh, scale=0.5), reads=["dd"], writes=["dd"])
                V(lambda e: e.tensor_scalar(out=w1[:], in0=dd[:], scalar1=1.0, scalar2=0.5, op0=ALU.add, op1=ALU.mult), ["dd"], ["w1"])
                V(lambda e: e.tensor_tensor(out=w1[:], in0=w1[:], in1=pg[:], op=ALU.mult), ["w1", "pg"], ["w1"])
                V(lambda e: e.tensor_tensor(out=w2[:], in0=pg[:], in1=w1[:], op=ALU.subtract), ["w1", "pg"], ["w2"])
                V(lambda e: e.tensor_scalar(out=cw8[:], in0=mask1[:], scalar1=w1[:, 0:1], scalar2=None, op0=ALU.mult), ["mask1", "w1"], ["cw8"])
                V(lambda e: e.scalar_tensor_tensor(out=cw8[:], in0=mask2[:], scalar=w2[:, 0:1], in1=cw8[:], op0=ALU.mult, op1=ALU.add), ["mask2", "w2", "cw8"], ["cw8"])
                for g in range(4):
                    V(lambda e, g=g: e.tensor_scalar(out=comb[:, 8 * g:8 * g + 8], in0=cw8[:], scalar1=gmask[:, g:g + 1], scalar2=None, op0=ALU.mult),
                      ["cw8", "gmask"], ["comb"])
                S_.op("pe", (lambda e, blk=blk: e.transpose(ps[bt][0:32, blk * 128:(blk + 1) * 128], comb[:, :], ident)),
                      reads=["comb", "colp"], writes=[f"ps{bt}"])
            S_.op("act", lambda e: e.activation(out=combT[:], in_=ps[bt][0:32, :], func=AF.Copy), reads=[f"ps{bt}"], writes=["combT"])

        def moe(t, l):
            norm_to_h(gs2[:, l, :], mod[:, l, 24:32])
            router(l)
            BA, BB, BC = (0, 1), (2, 3), (4, 5, 6, 7)
            g2c = lambda dc: mod[:, l, 40 + dc:41 + dc]
            dctr = [0]

            def load_cbc(e_):
                j = e_ % NCB
                S_.dma("sp", cbc[j][:], combT_d[e_:e_ + 1, :].broadcast_to([128, TT]), f"s_cbc{j}", reads=["combT_d"], writes=[f"cbc{j}"])

            def gate_up(e_):
                i = e_ % NGU
                j = e_ % 2
                a = e_ % NACT
                c = e_ % NCB
                if e_ + 2 < NE:
                    load_cbc(e_ + 2)
                for hc, (bg, bu) in enumerate((BA, BB)):
                    mm_group(bg, [(egu[i][:, kc, hc * 128:(hc + 1) * 128], h[:, kc, :]) for kc in range(NCH)], reads=[f"eg{i}"] + HK)
                    mm_group(bu, [(egu[i][:, kc, 256 + hc * 128:256 + (hc + 1) * 128], h[:, kc, :]) for kc in range(NCH)], reads=[f"eu{i}"] + HK)
                    S_.op("act", (lambda e, bg=bg, hc=hc, j=j: e.activation(out=sgt[j][:, hc, :], in_=ps[bg][:], func=AF.Silu)),
                          reads=[f"ps{bg}"], writes=[f"sgt{j}_{hc}"])
                    S_.op("dve", (lambda e, bu=bu, hc=hc, j=j, c=c: e.tensor_tensor(out=tbt[j][:, hc, :], in0=ps[bu][:], in1=cbc[c][:], op=ALU.mult)),
                          reads=[f"ps{bu}", f"cbc{c}"], writes=[f"tbt{j}_{hc}"])
                for hc in range(2):
                    S_.op("dve", (lambda e, hc=hc, j=j, a=a: e.tensor_tensor(out=actt[a][:, hc, :], in0=tbt[j][:, hc, :], in1=sgt[j][:, hc, :], op=ALU.mult)),
                          reads=[f"tbt{j}_{hc}", f"sgt{j}_{hc}"], writes=[f"actt{a}_{hc}"])
                if e_ + NGU < NE:
                    load_egu(l, e_ + NGU)

            def down_pair(p):
                es = (2 * p, 2 * p + 1)
                for dc in range(NCH):
                    bd = BC[dctr[0] % len(BC)]
                    dctr[0] += 1
                    pairs, rd = [], []
                    for e_ in es:
                        for hc in range(2):
                            pairs.append((ed[e_ % NED][:, hc, dc * 128:(dc + 1) * 128], actt[e_ % NACT][:, hc, :]))
                        rd += [f"ed{e_ % NED}", f"actt{e_ % NACT}_0", f"actt{e_ % NACT}_1"]
                    mm_group(bd, pairs, reads=rd)
                    S_.op("dve", (lambda e, bd=bd, dc=dc: e.scalar_tensor_tensor(out=x[:, dc, :], in0=ps[bd][:], scalar=g2c(dc), in1=x[:, dc, :],
                                                                                 op0=ALU.mult, op1=ALU.add)),
                          reads=[f"ps{bd}", XK[dc], "mod"], writes=[XK[dc]])
                for e_ in es:
                    if e_ + NED < NE:
                        load_ed(l, e_ + NED)

            S_.dma("sp", combT_d, combT[:], "s_combd", reads=["combT"], writes=["combT_d"])
            load_cbc(0)
            load_cbc(1)
            gate_up(0)
            gate_up(1)
            for p in range(NE // 2):
                if 2 * p + 2 < NE:
                    gate_up(2 * p + 2)
                    gate_up(2 * p + 3)
                down_pair(p)

        xv = xT.rearrange("(dc p) s -> p dc s", p=128)
        yv = yT.rearrange("(dc p) s -> p dc s", p=128)
        for t in range(NT):
            S_.dma("sp", x[:], xv[:, :, t * TT:(t + 1) * TT], "s_x", writes=XK)
            for l in range(first_layer, first_layer + NL):
                import os as _os
                _ph = _os.environ.get("K_PHASES", "mixer,moe")
                if "mixer" in _ph:
                    mixer(t, l)
                if "moe" in _ph:
                    moe(t, l)
            if final_norm:
                rms_stats()
                for dc in range(NCH):
                    S_.op("dve", (lambda e, dc=dc: e.scalar_tensor_tensor(out=x[:, dc, :], in0=x[:, dc, :], scalar=colp[:, C_FNG + dc:C_FNG + dc + 1], in1=rstd[:],
                                                                          op0=ALU.mult, op1=ALU.mult)),
                          reads=[XK[dc], "rstd", "colp"], writes=[XK[dc]])
            S_.dma("sp", yv[:, :, t * TT:(t + 1) * TT], x[:], "s_y", reads=XK)
        S_.final_wait("sp", ["s_y"])
        print("[kernel] sbuf bytes remaining/partition:", nc.sbuf_bytes_remaining)
        S_.emit()
    return nc


def _col(v):
    return np.ascontiguousarray(np.asarray(v, np.float32).reshape(-1, 128).T)


def _prep(inputs, S):
    f = lambda k: np.asarray(inputs[k], np.float32)
    B = f("x").shape[0]
    shared = {
        "w_ada": f("w_ada"), "w_in": f("w_in"), "w_co": f("w_conv_out"),
        "w_pool": np.ascontiguousarray(f("pool_w").reshape(2, 512, 256)),
        "w_out": f("w_out"),
        "w_r": np.ascontiguousarray(np.concatenate([f("w_router_group"), f("w_router_expert")], axis=-1)),
        "w_g": f("w_expert_gate"), "w_u": f("w_expert_up"), "w_d": f("w_expert_down"),
    }
    rowp = np.zeros((128, NROW), np.float32)
    for l in range(2):
        rb = np.concatenate([f("b_router_group")[l], f("b_router_expert")[l]])
        rowp[:, l * 36:(l + 1) * 36] = np.broadcast_to(rb[None, :], (128, 36))
    base = np.zeros((128, NCOL), np.float32)
    base[:, C_IDENT:C_IDENT + 128] = np.eye(128, dtype=np.float32)
    base[:, C_FNG:C_FNG + 8] = _col(f("final_norm_g"))
    base[:, C_INVC:C_INVC + 16] = np.broadcast_to((1.0 / np.arange(1, 17, dtype=np.float32))[None, :], (128, 16))
    for l in range(2):
        o = C_L0 + l * PL
        base[:, o + O_MNG:o + O_MNG + 8] = _col(f("mixer_norm_g")[l])
        base[:, o + O_BADA:o + O_BADA + 48] = _col(f("b_ada")[l])
        cw = f("conv_w")[l]
        for k in range(CONV_K):
            base[:, o + O_CONVW + 4 * k:o + O_CONVW + 4 * k + 4] = _col(cw[k])
        base[:, o + O_CONVB:o + O_CONVB + 4] = _col(f("conv_b")[l])
        base[:, o + O_LNG:o + O_LNG + 4] = _col(f("conv_ln_g")[l])
        base[:, o + O_LNB:o + O_LNB + 4] = _col(f("conv_ln_b")[l])
        base[:, o + O_BCO:o + O_BCO + 8] = _col(f("b_conv_out")[l])
        base[:, o + O_PSC:o + O_PSC + 8] = _col(f("pool_scale")[l])
        base[:, o + O_FNG2:o + O_FNG2 + 8] = _col(f("ffn_norm_g")[l])
    in_maps = []
    for b in range(B):
        cp = base.copy()
        cp[:, C_C:C_C + 8] = _col(f("c")[b])
        m = dict(shared)
        m["colp"] = cp
        m["rowp"] = rowp
        m["xT"] = np.ascontiguousarray(f("x")[b, :S, :].T)
        in_maps.append(m)
    return in_maps


_NC_CACHE = {}


def _get_nc(S, NL, final_norm, first_layer):
    key = (S, NL, final_norm, first_layer)
    if key not in _NC_CACHE:
        _NC_CACHE[key] = build_nc(S, NL, final_norm, first_layer)
    return _NC_CACHE[key]


def run(inputs, S=4096, NL=2, final_norm=True, first_layer=0, trace=False):
    in_maps = _prep(inputs, S)
    nc = _get_nc(S, NL, final_norm, first_layer)
    res = run_bass_kernel_spmd(nc, in_maps, core_ids=list(range(len(in_maps))), trace=trace)
    out = np.stack([np.ascontiguousarray(r["yT"].T) for r in res.results], axis=0)
    return out, res


def kernel(**inputs):
    out, _ = run(inputs, S=4096, NL=2, final_norm=True)
    return out.astype(np.float32)
```

```python
import contextlib
import numpy as np
import concourse.bass as bass
import concourse.mybir as mybir
from concourse.bass_utils import run_bass_kernel_spmd

F32 = mybir.dt.float32
BF16 = mybir.dt.bfloat16
AF = mybir.ActivationFunctionType
ALU = mybir.AluOpType
AX = mybir.AxisListType

D = 1024
NCH = 8
TT = 512
CONV_K = 31
HV = CONV_K - 1
HU = 15
POOL_W = (2, 4, 8, 16)
NE = 32
EPS = 1e-6

C_IDENT = 0
C_C = 128
C_FNG = 136
C_INVC = 144
C_L0 = 160
PL = 216
O_MNG, O_BADA, O_CONVW, O_CONVB, O_LNG, O_LNB, O_BCO, O_PSC, O_FNG2 = 0, 8, 56, 180, 184, 188, 192, 200, 208
NCOL = C_L0 + 2 * PL
NROW = 2 * 36

ENGS = ("pe", "act", "dve", "pool", "sp")


class Sched:
    def __init__(self, nc):
        self.nc = nc
        self.ops = {e: [] for e in ENGS}
        self.semval = {}
        self.seen = {e: {} for e in ENGS}
        self.buf = {}
        self.own = {e: "c_" + e for e in ENGS}
        for e in ENGS:
            self.semval[self.own[e]] = 0

    def _b(self, k):
        if k not in self.buf:
            self.buf[k] = [None, {}]
        return self.buf[k]

    def op(self, eng, fn, reads=(), writes=(), sem=None, inc=1):
        own = self.own[eng]
        deps = {}

        def add(s, v, kind):
            if s == own and sem is None and eng == "pe":
                return
            if deps.get(s, 0) < v:
                deps[s] = v

        for b in reads:
            w = self._b(b)[0]
            if w:
                add(w[0], w[1], "raw")
            if b.startswith("ps"):
                for s, v in self._b(b)[1].items():
                    if s != own:
                        add(s, v, "psrd")
        for b in writes:
            st = self._b(b)
            if st[0]:
                add(st[0][0], st[0][1], "waw")
            for s, v in st[1].items():
                add(s, v, "war")
        waits = []
        for s, v in deps.items():
            if self.seen[eng].get(s, 0) < v:
                self.seen[eng][s] = v
                waits.append((s, v))
        semname = sem or own
        if semname not in self.semval:
            self.semval[semname] = 0
        if inc:
            self.semval[semname] += inc
            val = self.semval[semname]
        else:
            val = self.semval[semname] + 1
        self.ops[eng].append((waits, fn, semname, inc))
        for b in reads:
            st = self._b(b)
            if st[1].get(semname, 0) < val:
                st[1][semname] = val
        for b in writes:
            st = self._b(b)
            st[0] = (semname, val)
            st[1] = {}
        return val

    def dma(self, eng, out, in_, sem, reads=(), writes=()):
        return self.op(eng, lambda e: e.dma_start(out=out, in_=in_), reads=reads, writes=writes, sem=sem, inc=16)

    def final_wait(self, eng, sems):
        waits = [(s, self.semval[s]) for s in sems if self.semval.get(s, 0) > 0]
        self.ops[eng].append((waits, None, None, 0))

    def emit(self):
        nc = self.nc
        with contextlib.ExitStack() as st:
            handles = {}
            for name in self.semval:
                handles[name] = st.enter_context(nc.semaphore(name))
            block = st.enter_context(nc.Block())

            def run(engobj, lst):
                for waits, fn, semname, inc in lst:
                    for s, v in waits:
                        engobj.wait_ge(handles[s], v)
                    if fn is None:
                        continue
                    ins = fn(engobj)
                    if inc:
                        ins.then_inc(handles[semname], inc)

            @block.tensor
            def _(e):
                run(e, self.ops["pe"])

            @block.scalar
            def _(e):
                run(e, self.ops["act"])

            @block.vector
            def _(e):
                run(e, self.ops["dve"])

            @block.gpsimd
            def _(e):
                run(e, self.ops["pool"])

            @block.sync
            def _(e):
                run(e, self.ops["sp"])


def build_nc(S=4096, NL=2, final_norm=True, first_layer=0):
    NT = S // TT
    nc = bass.Bass("TRN2", target_bir_lowering=False)

    def din(name, shape):
        return nc.dram_tensor(name, list(shape), F32, kind="ExternalInput").ap()

    xT = din("xT", [D, S])
    colp_d = din("colp", [128, NCOL])
    rowp_d = din("rowp", [128, NROW])
    w_ada = din("w_ada", [2, D, 6 * D])
    w_in = din("w_in", [2, D, 3584])
    w_co = din("w_co", [2, 512, D])
    w_pool = din("w_pool", [2, 512, 256])
    w_out = din("w_out", [2, D, D])
    w_r = din("w_r", [2, D, 36])
    w_g = din("w_g", [2, NE, D, 256])
    w_u = din("w_u", [2, NE, D, 256])
    w_d = din("w_d", [2, NE, 256, D])
    yT = nc.dram_tensor("yT", [D, S], F32, kind="ExternalOutput").ap()
    combT_d = nc.dram_tensor("combT_d", [32, TT], BF16, kind="Internal").ap()

    S_ = Sched(nc)
    st = contextlib.ExitStack()
    with st:
        def sb(name, shape, dt):
            return st.enter_context(nc.sbuf_tensor(name, list(shape), dt))

        colp = sb("colp_s", [128, NCOL], F32)
        rowp = sb("rowp_s", [128, NROW], F32)
        x = sb("x", [128, NCH, TT], F32)
        h = sb("h", [128, NCH, TT], BF16)
        mix = sb("mix", [128, NCH, TT], BF16)
        NTMP = 4
        tmp = [sb(f"tmp{i}", [128, TT], F32) for i in range(NTMP)]
        rstd = sb("rstd", [128, TT], F32)
        stat = [sb(f"stat{i}", [128, TT], F32) for i in range(3)]
        vb = sb("vb", [128, 4, HV + TT], BF16)
        vhalo = sb("vhalo", [128, 2, 4, HV], BF16)
        U = sb("U", [128, 4, HU + TT], F32)
        uhalo = sb("uhalo", [128, 2, 4, HU], F32)
        T1 = sb("T1", [128, HU + TT], F32)
        T2 = sb("T2", [128, HU + TT], F32)
        pooled = sb("pooled", [128, 4, TT], BF16)
        cvo = sb("cvo", [128, 4, TT], F32)
        cv = sb("cv", [128, 4, TT], BF16)
        NDG = 2
        dg = [sb(f"dg{i}", [128, CONV_K, 128], BF16) for i in range(NDG)]
        NWS = 4
        wsl = [sb(f"wsl{i}", [128, NCH, 512], BF16) for i in range(NWS)]
        wco = sb("wco", [128, 4, D], BF16)
        pw = sb("pw", [128, 4, 256], BF16)
        wr = sb("wr", [128, 2, NCH, 36], BF16)
        NGU = 3
        NED = 4
        egu = [sb(f"egu{i}", [128, NCH, 512], BF16) for i in range(NGU)]
        ed = [sb(f"ed{i}", [128, 2, D], BF16) for i in range(NED)]
        sgt = [sb(f"sgt{i}", [128, 2, TT], BF16) for i in range(2)]
        tbt = [sb(f"tbt{i}", [128, 2, TT], BF16) for i in range(2)]
        NACT = 4
        actt = [sb(f"actt{i}", [128, 2, TT], BF16) for i in range(NACT)]
        NCB = 3
        cbc = [sb(f"cbc{i}", [128, TT], BF16) for i in range(NCB)]
        combT = sb("combT", [32, TT], BF16)
        ident_bf = sb("ident_bf", [128, 128], BF16)
        ones_bf = sb("ones_bf", [128, 128], BF16)
        epsT = sb("epsT", [128, 1], F32)
        cact = sb("cact", [128, NCH], BF16)
        cwb = sb("cwb", [128, 2, 4 * CONV_K], BF16)
        mod = sb("mod", [128, 2, 48], F32)
        gs1 = sb("gs1", [128, 2, NCH], F32)
        gs2 = sb("gs2", [128, 2, NCH], F32)
        rl = sb("rl", [128, 4, 36], F32)
        gmask, d4, e4, den4 = [sb(f"r4_{i}", [128, 4, 4], F32) for i in range(4)]
        sel8, mask1, sel8b, mask2, cw8 = [sb(f"r8_{i}", [128, 4, 8], F32) for i in range(5)]
        gmax, ssum, pg, m1, m2, dd, w1, w2 = [sb(f"r1_{i}", [128, 4], F32) for i in range(8)]
        prod = sb("prod", [128, 4, 4, 8], F32)
        comb = sb("comb", [128, 4, 4, 8], F32)
        ps = [st.enter_context(nc.psum_tensor(f"ps{i}", [128, TT], F32)) for i in range(8)]

        ident = colp[:, C_IDENT:C_IDENT + 128]

        bank_ctr = [0]

        def bank():
            b = bank_ctr[0] % 8
            bank_ctr[0] += 1
            return b

        tmp_ctr = [0]

        def newtmp():
            i = tmp_ctr[0] % NTMP
            tmp_ctr[0] += 1
            return i

        def lcol(l, off, n=1):
            c = C_L0 + l * PL + off
            return colp[:, c:c + n]

        def mm_group(b, pairs, reads, M=128, N=TT, out=None):
            n = len(pairs)
            o = out if out is not None else ps[b][0:M, 0:N]
            for i, (lt, rh) in enumerate(pairs):
                S_.op("pe", (lambda e, lt=lt, rh=rh, i=i: e.matmul(o, lhsT=lt, rhs=rh, start=(i == 0), stop=(i == n - 1))),
                      reads=reads, writes=[f"ps{b}"], inc=1 if i == n - 1 else 0)

        wblocks = []
        for t_ in range(NT):
            for l_ in range(first_layer, first_layer + NL):
                wl_ = w_in[l_].rearrange("(kc p) n -> p kc n", p=128)
                wo_ = w_out[l_].rearrange("(kc p) n -> p kc n", p=128)
                for c0 in (0, 512, 1024, 1536, 2560, 2048, 3072):
                    wblocks.append(wl_[:, :, c0:c0 + 512])
                wblocks.append(wo_[:, :, 0:512])
                wblocks.append(wo_[:, :, 512:1024])
        ws_loaded = [0]

        def prefetch_to(k):
            while ws_loaded[0] < min(k, len(wblocks)):
                n = ws_loaded[0]
                i = n % NWS
                S_.dma("pool", wsl[i][:], wblocks[n], f"s_ws{i}", writes=[f"ws{i}"])
                ws_loaded[0] += 1

        mix_it = [0]

        S_.dma("sp", colp[:], colp_d, "s_colp", writes=["colp"])
        S_.dma("sp", rowp[:], rowp_d, "s_rowp", writes=["rowp"])
        S_.dma("pool", wr[:], w_r.rearrange("l (kc p) n -> p l kc n", p=128), "s_wr", writes=["wr"])
        S_.op("dve", lambda e: e.memset(ones_bf[:], 1.0), writes=["ones"])
        S_.op("dve", lambda e: e.memset(epsT[:], EPS), writes=["eps"])
        S_.op("dve", lambda e: e.tensor_copy(out=ident_bf[:], in_=ident), reads=["colp"], writes=["identbf"])
        for l_ in range(2):
            S_.op("dve", (lambda e, l_=l_: e.tensor_copy(out=cwb[:, l_, :], in_=lcol(l_, O_CONVW, 4 * CONV_K))), reads=["colp"], writes=["cwb"])
        S_.op("dve", lambda e: e.memset(vhalo[:], 0.0), writes=["vhalo"])
        S_.op("dve", lambda e: e.memset(uhalo[:], 0.0), writes=["uhalo"])
        S_.op("act", lambda e: e.activation(out=cact[:], in_=colp[:, C_C:C_C + NCH], func=AF.Silu), reads=["colp"], writes=["cact"])

        for l in range(first_layer, first_layer + NL):
            bm = bank()
            for j in range(24):
                sl = j % (2 * NGU)
                stage = egu[sl // 2][:, :, (sl % 2) * 256:(sl % 2 + 1) * 256]
                key = ("eg%d" if sl % 2 == 0 else "eu%d") % (sl // 2)
                S_.dma("pool", stage, w_ada[l, :, j * 256:(j + 1) * 256].rearrange("(kc p) n -> p kc n", p=128),
                       "s_" + key, writes=[key])
                for half in range(2):
                    cj = 2 * j + half
                    for kc in range(NCH):
                        S_.op("pe", (lambda e, stage=stage, half=half, kc=kc, cj=cj, bm=bm:
                                     e.matmul(ps[bm][:, cj:cj + 1], lhsT=stage[:, kc, half * 128:(half + 1) * 128],
                                              rhs=cact[:, kc:kc + 1], start=(kc == 0), stop=(kc == NCH - 1))),
                              reads=[key, "cact"], writes=[f"ps{bm}"], inc=1 if kc == NCH - 1 else 0)
            S_.op("dve", (lambda e, l=l, bm=bm: e.tensor_tensor(out=mod[:, l, :], in0=ps[bm][:, 0:48], in1=lcol(l, O_BADA, 48), op=ALU.add)),
                  reads=[f"ps{bm}", "colp"], writes=["mod"])
            S_.op("dve", (lambda e, l=l: e.scalar_tensor_tensor(out=gs1[:, l, :], in0=mod[:, l, 8:16], scalar=1.0, in1=lcol(l, O_MNG, 8),
                                                                 op0=ALU.add, op1=ALU.mult)),
                  reads=["mod", "colp"], writes=["gs1"])
            S_.op("dve", (lambda e, l=l: e.scalar_tensor_tensor(out=gs2[:, l, :], in0=mod[:, l, 32:40], scalar=1.0, in1=lcol(l, O_FNG2, 8),
                                                                 op0=ALU.add, op1=ALU.mult)),
                  reads=["mod", "colp"], writes=["gs2"])

        XK = [f"x{dc}" for dc in range(NCH)]
        HK = [f"h{dc}" for dc in range(NCH)]
        MK = [f"mix{dc}" for dc in range(NCH)]

        def rms_stats():
            for dc in range(NCH):
                S_.op("act", (lambda e, dc=dc: e.activation(out=mix[:, dc, :], in_=x[:, dc, :], func=AF.Square)),
                      reads=[XK[dc]], writes=[MK[dc]])
            b = bank()
            mm_group(b, [(ones_bf[:], mix[:, dc, :]) for dc in range(NCH)], reads=["ones"] + MK)
            S_.op("act", (lambda e, b=b: e.activation(out=rstd[:], in_=ps[b][:], func=AF.Sqrt, bias=epsT[:, 0:1], scale=1.0 / D)),
                  reads=[f"ps{b}", "eps"], writes=["rstd"])
            S_.op("dve", lambda e: e.reciprocal(out=rstd[:], in_=rstd[:]), reads=["rstd"], writes=["rstd"])

        def norm_to_h(gs_ap, sh_ap):
            rms_stats()
            for dc in range(NCH):
                ti = newtmp()
                S_.op("dve", (lambda e, dc=dc, ti=ti: e.tensor_tensor(out=tmp[ti][:], in0=x[:, dc, :], in1=rstd[:], op=ALU.mult)),
                      reads=[XK[dc], "rstd"], writes=[f"tmp{ti}"])
                S_.op("act", (lambda e, dc=dc, ti=ti: e.activation(out=h[:, dc, :], in_=tmp[ti][:], func=AF.Identity,
                                                                   bias=sh_ap[:, dc:dc + 1], scale=gs_ap[:, dc:dc + 1])),
                      reads=[f"tmp{ti}", "mod", "gs1", "gs2"], writes=[HK[dc]])

        dg_ctr = [0]

        def build_dg(l, cc):
            di = cc % NDG
            S_.op("dve", (lambda e, di=di, cc=cc: e.tensor_tensor(
                out=dg[di][:, :, :], in0=ident_bf[:].unsqueeze(1).broadcast_to([128, CONV_K, 128]),
                in1=cwb[:, l, cc * CONV_K:(cc + 1) * CONV_K].unsqueeze(2).broadcast_to([128, CONV_K, 128]), op=ALU.mult)),
                reads=["identbf", "cwb"], writes=[f"dg{di}"])

        def mixer(t, l):
            first = (t == 0)
            for cc in range(NDG):
                build_dg(l, cc)
            norm_to_h(gs1[:, l, :], mod[:, l, 0:8])
            base = 9 * mix_it[0]
            mix_it[0] += 1
            blk = lambda n: (base + n) % NWS
            import os as _os2
            _stop = int(_os2.environ.get("K_MIX_STOP", "9"))
            if _stop <= 1:
                return
            prefetch_to(base + 0 + NWS)
            S_.dma("pool", wco[:], w_co[l].rearrange("(cc p) n -> p cc n", p=128), "s_wco", writes=["wco"])
            S_.dma("pool", pw[:], w_pool[l].rearrange("(g p) n -> p g n", p=128), "s_pw", writes=["pw"])
            iA, iG, iU = blk(0), blk(1), blk(2)
            S_.op("act", (lambda e: e.activation(out=vb[:, :, 0:HV], in_=vhalo[:, l, :, :], func=AF.Copy)), reads=["vhalo"], writes=["vb"])
            for cc in range(4):
                ba = bank()
                mm_group(ba, [(wsl[iA][:, kc, cc * 128:(cc + 1) * 128], h[:, kc, :]) for kc in range(NCH)], reads=[f"ws{iA}"] + HK)
                bg = bank()
                mm_group(bg, [(wsl[iG][:, kc, cc * 128:(cc + 1) * 128], h[:, kc, :]) for kc in range(NCH)], reads=[f"ws{iG}"] + HK)
                ti = newtmp()
                S_.op("act", (lambda e, bg=bg, ti=ti: e.activation(out=tmp[ti][:], in_=ps[bg][:], func=AF.Sigmoid)),
                      reads=[f"ps{bg}"], writes=[f"tmp{ti}"])
                S_.op("dve", (lambda e, ba=ba, ti=ti, cc=cc: e.tensor_tensor(out=vb[:, cc, HV:HV + TT], in0=ps[ba][:], in1=tmp[ti][:], op=ALU.mult)),
                      reads=[f"ps{ba}", f"tmp{ti}"], writes=["vb"])
            S_.op("act", (lambda e: e.activation(out=vhalo[:, l, :, :], in_=vb[:, :, TT:TT + HV], func=AF.Copy)), reads=["vb"], writes=["vhalo"])
            if _stop <= 2:
                return
            prefetch_to(base + 2 + NWS)
            S_.op("act", (lambda e: e.activation(out=U[:, :, 0:HU], in_=uhalo[:, l, :, :], func=AF.Copy)), reads=["uhalo"], writes=["U"])
            for gi in range(4):
                b = bank()
                mm_group(b, [(wsl[iU][:, kc, gi * 128:(gi + 1) * 128], h[:, kc, :]) for kc in range(NCH)], reads=[f"ws{iU}"] + HK)
                S_.op("act", (lambda e, b=b, gi=gi: e.activation(out=U[:, gi, HU:HU + TT], in_=ps[b][:], func=AF.Copy)),
                      reads=[f"ps{b}"], writes=["U"])
            S_.op("act", (lambda e: e.activation(out=uhalo[:, l, :, :], in_=U[:, :, TT:TT + HU], func=AF.Copy)), reads=["U"], writes=["uhalo"])
            W_ = HU + TT
            for gi, w in enumerate(POOL_W):
                src, srck = U[:, gi, :], "U"
                lo = 0
                dsts = [(T1, "T1"), (T2, "T2")]
                for j in range(gi + 1):
                    sh = 1 << j
                    dst, dstk = dsts[j % 2]
                    S_.op("dve", (lambda e, dst=dst, src=src, lo=lo, sh=sh: e.tensor_tensor(
                        out=dst[:, lo + sh:W_], in0=src[:, lo + sh:W_], in1=src[:, lo:W_ - sh], op=ALU.add)),
                        reads=[srck], writes=[dstk])
                    src, srck = dst[:], dstk
                    lo += sh
                S_.op("dve", (lambda e, src=src, gi=gi, w=w: e.scalar_tensor_tensor(
                    out=pooled[:, gi, :], in0=src[:, HU:W_], scalar=1.0 / w, in1=U[:, gi, HU:W_], op0=ALU.mult, op1=ALU.subtract)),
                    reads=[srck, "U"], writes=["pooled"])
                if first:
                    n = w - 1
                    fxb, fxk = dsts[(gi + 1) % 2]
                    S_.op("dve", (lambda e, src=src, n=n, fxb=fxb: e.tensor_tensor(out=fxb[:, 0:n], in0=src[:, HU:HU + n],
                                                                                   in1=colp[:, C_INVC:C_INVC + n], op=ALU.mult)),
                          reads=[srck, "colp"], writes=[fxk])
                    S_.op("dve", (lambda e, fxb=fxb, n=n, gi=gi: e.tensor_tensor(out=pooled[:, gi, 0:n], in0=fxb[:, 0:n], in1=U[:, gi, HU:HU + n], op=ALU.subtract)),
                          reads=[fxk, "U"], writes=["pooled"])
            if _stop <= 3:
                return
            bs1 = bank()
            bs2 = bank()
            for cc in range(4):
                di = cc % NDG
                _skip = ""
                if cc >= NDG:
                    build_dg(l, cc)
                b = bank()
                _nk = int(_os2.environ.get("K_CONV_NK", str(CONV_K)))
                if "m" not in _skip:
                    mm_group(b, [(dg[di][:, k, :], vb[:, cc, k:k + TT]) for k in range(_nk)], reads=[f"dg{di}", "vb"])
                if "e" in _skip:
                    continue
                S_.op("act", (lambda e, b=b, cc=cc: e.activation(out=cvo[:, cc, :], in_=ps[b][:], func=AF.Identity, bias=lcol(l, O_CONVB + cc), scale=1.0)),
                      reads=[f"ps{b}", "colp"], writes=[f"cvo{cc}"])
                S_.op("dve", (lambda e, cc=cc: e.tensor_copy(out=mix[:, cc, :], in_=cvo[:, cc, :])), reads=[f"cvo{cc}"], writes=[MK[cc]])
                S_.op("act", (lambda e, cc=cc: e.activation(out=mix[:, 4 + cc, :], in_=cvo[:, cc, :], func=AF.Square)),
                      reads=[f"cvo{cc}"], writes=[MK[4 + cc]])
            _sub = int(_os2.environ.get("K_CONV_SUB", "9"))
            if _sub <= 1:
                return
            mm_group(bs1, [(ones_bf[:], mix[:, cc, :]) for cc in range(4)], reads=["ones"] + MK[0:4])
            mm_group(bs2, [(ones_bf[:], mix[:, 4 + cc, :]) for cc in range(4)], reads=["ones"] + MK[4:8])
            S_.op("dve", (lambda e: e.tensor_scalar(out=stat[0][:], in0=ps[bs1][:], scalar1=1.0 / 512, scalar2=None, op0=ALU.mult)),
                  reads=[f"ps{bs1}"], writes=["stat0"])
            S_.op("dve", (lambda e: e.tensor_tensor(out=stat[1][:], in0=stat[0][:], in1=stat[0][:], op=ALU.mult)), reads=["stat0"], writes=["stat1"])
            S_.op("dve", (lambda e: e.scalar_tensor_tensor(out=stat[1][:], in0=ps[bs2][:], scalar=1.0 / 512, in1=stat[1][:], op0=ALU.mult, op1=ALU.subtract)),
                  reads=[f"ps{bs2}", "stat1"], writes=["stat1"])
            S_.op("act", (lambda e: e.activation(out=stat[2][:], in_=stat[1][:], func=AF.Sqrt, bias=epsT[:, 0:1], scale=1.0)),
                  reads=["stat1", "eps"], writes=["stat2"])
            S_.op("dve", (lambda e: e.reciprocal(out=stat[2][:], in_=stat[2][:])), reads=["stat2"], writes=["stat2"])
            if _sub <= 2:
                return
            for cc in range(4):
                S_.op("dve", (lambda e, cc=cc: e.tensor_tensor(out=cvo[:, cc, :], in0=cvo[:, cc, :], in1=stat[0][:], op=ALU.subtract)),
                      reads=[f"cvo{cc}", "stat0"], writes=[f"cvo{cc}"])
                S_.op("dve", (lambda e, cc=cc: e.tensor_tensor(out=cvo[:, cc, :], in0=cvo[:, cc, :], in1=stat[2][:], op=ALU.mult)),
                      reads=[f"cvo{cc}", "stat2"], writes=[f"cvo{cc}"])
                S_.op("act", (lambda e, cc=cc: e.activation(out=cv[:, cc, :], in_=cvo[:, cc, :], func=AF.Silu, bias=lcol(l, O_LNB + cc), scale=lcol(l, O_LNG + cc))),
                      reads=[f"cvo{cc}", "colp"], writes=[f"cv{cc}"])
            if _stop <= 4:
                return
            CVK = [f"cv{cc}" for cc in range(4)]
            preload_experts(l)
            for half in range(2):
                prefetch_to(base + 3 + 2 * half + NWS)
                iGA, iGB = blk(3 + 2 * half), blk(4 + 2 * half)
                for q in range(4):
                    dc = half * 4 + q
                    bya = bank()
                    mm_group(bya, [(wco[:, cc, dc * 128:(dc + 1) * 128], cv[:, cc, :]) for cc in range(4)], reads=["wco"] + CVK)
                    byb = bank()
                    g = dc // 2
                    mm_group(byb, [(pw[:, g, (dc % 2) * 128:(dc % 2 + 1) * 128], pooled[:, g, :])], reads=["pw", "pooled"])
                    bga = bank()
                    mm_group(bga, [(wsl[iGA][:, kc, q * 128:(q + 1) * 128], h[:, kc, :]) for kc in range(NCH)], reads=[f"ws{iGA}"] + HK)
                    bgb = bank()
                    mm_group(bgb, [(wsl[iGB][:, kc, q * 128:(q + 1) * 128], h[:, kc, :]) for kc in range(NCH)], reads=[f"ws{iGB}"] + HK)
                    ta, tb_ = newtmp(), newtmp()
                    S_.op("act", (lambda e, bga=bga, ta=ta: e.activation(out=tmp[ta][:], in_=ps[bga][:], func=AF.Sigmoid)), reads=[f"ps{bga}"], writes=[f"tmp{ta}"])
                    S_.op("act", (lambda e, bgb=bgb, tb_=tb_: e.activation(out=tmp[tb_][:], in_=ps[bgb][:], func=AF.Sigmoid)), reads=[f"ps{bgb}"], writes=[f"tmp{tb_}"])
                    S_.op("dve", (lambda e, bya=bya, ta=ta, dc=dc: e.scalar_tensor_tensor(out=tmp[ta][:], in0=ps[bya][:], scalar=lcol(l, O_BCO + dc), in1=tmp[ta][:],
                                                                                           op0=ALU.add, op1=ALU.mult)),
                          reads=[f"ps{bya}", f"tmp{ta}", "colp"], writes=[f"tmp{ta}"])
                    S_.op("dve", (lambda e, byb=byb, tb_=tb_, dc=dc: e.scalar_tensor_tensor(out=tmp[tb_][:], in0=ps[byb][:], scalar=lcol(l, O_PSC + dc), in1=tmp[tb_][:],
                                                                                             op0=ALU.mult, op1=ALU.mult)),
                          reads=[f"ps{byb}", f"tmp{tb_}", "colp"], writes=[f"tmp{tb_}"])
                    S_.op("dve", (lambda e, ta=ta, tb_=tb_, dc=dc: e.tensor_tensor(out=mix[:, dc, :], in0=tmp[ta][:], in1=tmp[tb_][:], op=ALU.add)),
                          reads=[f"tmp{ta}", f"tmp{tb_}"], writes=[MK[dc]])
            if _stop <= 5:
                return
            for half in range(2):
                prefetch_to(base + 7 + half + NWS)
                iO = blk(7 + half)
                for q in range(4):
                    dc = half * 4 + q
                    b = bank()
                    mm_group(b, [(wsl[iO][:, kc, q * 128:(q + 1) * 128], mix[:, kc, :]) for kc in range(NCH)], reads=[f"ws{iO}"] + MK)
                    S_.op("dve", (lambda e, b=b, dc=dc: e.scalar_tensor_tensor(out=x[:, dc, :], in0=ps[b][:], scalar=mod[:, l, 16 + dc:17 + dc], in1=x[:, dc, :],
                                                                               op0=ALU.mult, op1=ALU.add)),
                          reads=[f"ps{b}", XK[dc], "mod"], writes=[XK[dc]])

        def load_egu(l, e_):
            i = e_ % NGU
            S_.dma("pool", egu[i][:, :, 0:256], w_g[l, e_].rearrange("(kc p) n -> p kc n", p=128), f"s_eg{i}", writes=[f"eg{i}"])
            S_.dma("pool", egu[i][:, :, 256:512], w_u[l, e_].rearrange("(kc p) n -> p kc n", p=128), f"s_eu{i}", writes=[f"eu{i}"])

        def load_ed(l, e_):
            i = e_ % NED
            S_.dma("pool", ed[i][:], w_d[l, e_].rearrange("(hc p) n -> p hc n", p=128), f"s_ed{i}", writes=[f"ed{i}"])

        def preload_experts(l):
            for e_ in range(NGU):
                load_egu(l, e_)
            for e_ in range(NED):
                load_ed(l, e_)

        NB = TT // 128

        def router_logits(l):
            b = 4
            for blk in range(NB):
                n = NCH
                for kc in range(NCH):
                    S_.op("pe", (lambda e, blk=blk, kc=kc, b=b: e.matmul(ps[b][:, blk * 36:(blk + 1) * 36], lhsT=h[:, kc, blk * 128:(blk + 1) * 128],
                                                                      rhs=wr[:, l, kc, :], start=(kc == 0), stop=(kc == NCH - 1))),
                          reads=HK + ["wr"], writes=[f"ps{b}"], inc=1 if kc == NCH - 1 else 0)
            return b

        def router_topk(l, b):
            V = lambda fn, reads, writes: S_.op("dve", fn, reads=reads, writes=writes)
            bc = lambda ap, shape: ap.broadcast_to(shape)
            rl3 = rl[:, :, :]
            gl = rl[:, :, 0:4]
            el = rl[:, :, 4:36].rearrange("p b (g e) -> p b g e", e=8)
            V(lambda e: e.tensor_tensor(out=rl3, in0=ps[b][:, 0:NB * 36].rearrange("p (b n) -> p b n", n=36),
                                        in1=bc(rowp[:, l * 36:(l + 1) * 36].unsqueeze(1), [128, NB, 36]), op=ALU.add), [f"ps{b}", "rowp"], ["rl"])
            V(lambda e: e.tensor_reduce(out=gmax[:], in_=gl, axis=AX.X, op=ALU.max), ["rl"], ["gmax"])
            V(lambda e: e.tensor_tensor(out=gmask[:], in0=gl, in1=bc(gmax[:].unsqueeze(2), [128, NB, 4]), op=ALU.is_equal), ["rl", "gmax"], ["gmask"])
            V(lambda e: e.tensor_tensor(out=d4[:], in0=gl, in1=bc(gmax[:].unsqueeze(2), [128, NB, 4]), op=ALU.subtract), ["rl", "gmax"], ["d4"])
            S_.op("act", lambda e: e.activation(out=d4[:], in_=d4[:], func=AF.Tanh, scale=0.5), reads=["d4"], writes=["d4"])
            V(lambda e: e.tensor_scalar(out=den4[:], in0=d4[:], scalar1=-1.0, scalar2=1.0, op0=ALU.mult, op1=ALU.add), ["d4"], ["den4"])
            V(lambda e: e.reciprocal(out=den4[:], in_=den4[:]), ["den4"], ["den4"])
            V(lambda e: e.scalar_tensor_tensor(out=e4[:], in0=d4[:], scalar=1.0, in1=den4[:], op0=ALU.add, op1=ALU.mult), ["d4", "den4"], ["e4"])
            V(lambda e: e.tensor_reduce(out=ssum[:], in_=e4[:], axis=AX.X, op=ALU.add), ["e4"], ["ssum"])
            V(lambda e: e.reciprocal(out=pg[:], in_=ssum[:]), ["ssum"], ["pg"])
            V(lambda e: e.tensor_tensor(out=prod[:], in0=el, in1=bc(gmask[:].unsqueeze(3), [128, NB, 4, 8]), op=ALU.mult), ["rl", "gmask"], ["prod"])
            V(lambda e: e.tensor_reduce(out=sel8[:], in_=prod[:].rearrange("p b g e -> p b e g"), axis=AX.X, op=ALU.add), ["prod"], ["sel8"])
            V(lambda e: e.tensor_reduce(out=m1[:], in_=sel8[:], axis=AX.X, op=ALU.max), ["sel8"], ["m1"])
            V(lambda e: e.tensor_tensor(out=mask1[:], in0=sel8[:], in1=bc(m1[:].unsqueeze(2), [128, NB, 8]), op=ALU.is_equal), ["sel8", "m1"], ["mask1"])
            V(lambda e: e.scalar_tensor_tensor(out=sel8b[:], in0=mask1[:], scalar=-1e30, in1=sel8[:], op0=ALU.mult, op1=ALU.add), ["mask1", "sel8"], ["sel8b"])
            V(lambda e: e.tensor_reduce(out=m2[:], in_=sel8b[:], axis=AX.X, op=ALU.max), ["sel8b"], ["m2"])
            V(lambda e: e.tensor_tensor(out=mask2[:], in0=sel8b[:], in1=bc(m2[:].unsqueeze(2), [128, NB, 8]), op=ALU.is_equal), ["sel8b", "m2"], ["mask2"])
            V(lambda e: e.tensor_tensor(out=dd[:], in0=m1[:], in1=m2[:], op=ALU.subtract), ["m1", "m2"], ["dd"])
            S_.op("act", lambda e: e.activation(out=dd[:], in_=dd[:], func=AF.Tanh, scale=0.5), reads=["dd"], writes=["dd"])
            V(lambda e: e.tensor_scalar(out=w1[:], in0=dd[:], scalar1=1.0, scalar2=0.5, op0=ALU.add, op1=ALU.mult), ["dd"], ["w1"])
            V(lambda e: e.tensor_tensor(out=w1[:], in0=w1[:], in1=pg[:], op=ALU.mult), ["w1", "pg"], ["w1"])
            V(lambda e: e.tensor_tensor(out=w2[:], in0=pg[:], in1=w1[:], op=ALU.subtract), ["w1", "pg"], ["w2"])
            V(lambda e: e.tensor_tensor(out=mask1[:], in0=mask1[:], in1=bc(w1[:].unsqueeze(2), [128, NB, 8]), op=ALU.mult), ["mask1", "w1"], ["mask1"])
            V(lambda e: e.tensor_tensor(out=mask2[:], in0=mask2[:], in1=bc(w2[:].unsqueeze(2), [128, NB, 8]), op=ALU.mult), ["mask2", "w2"], ["mask2"])
            V(lambda e: e.tensor_tensor(out=cw8[:], in0=mask1[:], in1=mask2[:], op=ALU.add), ["mask1", "mask2"], ["cw8"])
            V(lambda e: e.tensor_tensor(out=comb[:], in0=bc(cw8[:].unsqueeze(2), [128, NB, 4, 8]), in1=bc(gmask[:].unsqueeze(3), [128, NB, 4, 8]), op=ALU.mult),
              ["cw8", "gmask"], ["comb"])
            bt = 5
            for blk in range(NB):
                S_.op("pe", (lambda e, blk=blk: e.transpose(ps[bt][0:32, blk * 128:(blk + 1) * 128], comb[:, blk, :, :].rearrange("p g e -> p (g e)"), ident)),
                      reads=["comb", "colp"], writes=[f"ps{bt}"])
            S_.op("act", lambda e: e.activation(out=combT[:], in_=ps[bt][0:32, :], func=AF.Copy), reads=[f"ps{bt}"], writes=["combT"])

        def moe(t, l):
            norm_to_h(gs2[:, l, :], mod[:, l, 24:32])
            BA, BB, BC = (0, 1), (2, 3), (4, 5, 6, 7)
            g2c = lambda dc: mod[:, l, 40 + dc:41 + dc]
            dctr = [0]

            def load_cbc(e_):
                j = e_ % NCB
                S_.dma("sp", cbc[j][:], combT_d[e_:e_ + 1, :].broadcast_to([128, TT]), f"s_cbc{j}", reads=["combT_d"], writes=[f"cbc{j}"])

            def gate_up_pe(e_):
                i = e_ % NGU
                if e_ + 2 < NE:
                    load_cbc(e_ + 2)
                for hc, (bg, bu) in enumerate((BA, BB)):
                    mm_group(bg, [(egu[i][:, kc, hc * 128:(hc + 1) * 128], h[:, kc, :]) for kc in range(NCH)], reads=[f"eg{i}"] + HK)
                    mm_group(bu, [(egu[i][:, kc, 256 + hc * 128:256 + (hc + 1) * 128], h[:, kc, :]) for kc in range(NCH)], reads=[f"eu{i}"] + HK)
                if e_ + NGU < NE:
                    load_egu(l, e_ + NGU)

            def gate_up_ev(e_):
                j = e_ % 2
                a = e_ % NACT
                c = e_ % NCB
                for hc, (bg, bu) in enumerate((BA, BB)):
                    S_.op("act", (lambda e, bg=bg, hc=hc, j=j: e.activation(out=sgt[j][:, hc, :], in_=ps[bg][:], func=AF.Silu)),
                          reads=[f"ps{bg}"], writes=[f"sgt{j}_{hc}"])
                    S_.op("dve", (lambda e, bu=bu, hc=hc, j=j, c=c: e.tensor_tensor(out=tbt[j][:, hc, :], in0=ps[bu][:], in1=cbc[c][:], op=ALU.mult)),
                          reads=[f"ps{bu}", f"cbc{c}"], writes=[f"tbt{j}_{hc}"])
                for hc in range(2):
                    S_.op("dve", (lambda e, hc=hc, j=j, a=a: e.tensor_tensor(out=actt[a][:, hc, :], in0=tbt[j][:, hc, :], in1=sgt[j][:, hc, :], op=ALU.mult)),
                          reads=[f"tbt{j}_{hc}", f"sgt{j}_{hc}"], writes=[f"actt{a}_{hc}"])

            def gate_up(e_):
                gate_up_pe(e_)
                gate_up_ev(e_)

            def down_pair(p):
                es = (2 * p, 2 * p + 1)
                for dc in range(NCH):
                    bd = BC[dctr[0] % len(BC)]
                    dctr[0] += 1
                    pairs, rd = [], []
                    for e_ in es:
                        for hc in range(2):
                            pairs.append((ed[e_ % NED][:, hc, dc * 128:(dc + 1) * 128], actt[e_ % NACT][:, hc, :]))
                        rd += [f"ed{e_ % NED}", f"actt{e_ % NACT}_0", f"actt{e_ % NACT}_1"]
                    mm_group(bd, pairs, reads=rd)
                    S_.op("dve", (lambda e, bd=bd, dc=dc: e.scalar_tensor_tensor(out=x[:, dc, :], in0=ps[bd][:], scalar=g2c(dc), in1=x[:, dc, :],
                                                                                 op0=ALU.mult, op1=ALU.add)),
                          reads=[f"ps{bd}", XK[dc], "mod"], writes=[XK[dc]])
                for e_ in es:
                    if e_ + NED < NE:
                        load_ed(l, e_ + NED)

            rb = router_logits(l)
            i0 = 0 % NGU
            for hc, (bg, bu) in enumerate((BA, BB)):
                mm_group(bg, [(egu[i0][:, kc, hc * 128:(hc + 1) * 128], h[:, kc, :]) for kc in range(NCH)], reads=[f"eg{i0}"] + HK)
                mm_group(bu, [(egu[i0][:, kc, 256 + hc * 128:256 + (hc + 1) * 128], h[:, kc, :]) for kc in range(NCH)], reads=[f"eu{i0}"] + HK)
            router_topk(l, rb)
            S_.dma("sp", combT_d, combT[:], "s_combd", reads=["combT"], writes=["combT_d"])
            load_cbc(0)
            load_cbc(1)
            load_cbc(2)
            load_egu(l, NGU)
            gate_up_ev(0)
            gate_up(1)
            for p in range(NE // 2):
                if 2 * p + 2 < NE:
                    gate_up(2 * p + 2)
                    gate_up(2 * p + 3)
                down_pair(p)

        xv = xT.rearrange("(dc p) s -> p dc s", p=128)
        yv = yT.rearrange("(dc p) s -> p dc s", p=128)
        for t in range(NT):
            S_.dma("sp", x[:], xv[:, :, t * TT:(t + 1) * TT], "s_x", writes=XK)
            for l in range(first_layer, first_layer + NL):
                import os as _os
                _ph = _os.environ.get("K_PHASES", "mixer,moe")
                if "mixer" in _ph:
                    mixer(t, l)
                if "moe" in _ph:
                    moe(t, l)
            if final_norm:
                rms_stats()
                for dc in range(NCH):
                    S_.op("dve", (lambda e, dc=dc: e.scalar_tensor_tensor(out=x[:, dc, :], in0=x[:, dc, :], scalar=colp[:, C_FNG + dc:C_FNG + dc + 1], in1=rstd[:],
                                                                          op0=ALU.mult, op1=ALU.mult)),
                          reads=[XK[dc], "rstd", "colp"], writes=[XK[dc]])
            S_.dma("sp", yv[:, :, t * TT:(t + 1) * TT], x[:], "s_y", reads=XK)
        S_.final_wait("sp", ["s_y"])
        print("[kernel] sbuf bytes remaining/partition:", nc.sbuf_bytes_remaining)
        S_.emit()
    return nc


def _col(v):
    return np.ascontiguousarray(np.asarray(v, np.float32).reshape(-1, 128).T)


def _prep(inputs, S):
    f = lambda k: np.asarray(inputs[k], np.float32)
    B = f("x").shape[0]
    shared = {
        "w_ada": f("w_ada"), "w_in": f("w_in"), "w_co": f("w_conv_out"),
        "w_pool": np.ascontiguousarray(f("pool_w").reshape(2, 512, 256)),
        "w_out": f("w_out"),
        "w_r": np.ascontiguousarray(np.concatenate([f("w_router_group"), f("w_router_expert")], axis=-1)),
        "w_g": f("w_expert_gate"), "w_u": f("w_expert_up"), "w_d": f("w_expert_down"),
    }
    rowp = np.zeros((128, NROW), np.float32)
    for l in range(2):
        rb = np.concatenate([f("b_router_group")[l], f("b_router_expert")[l]])
        rowp[:, l * 36:(l + 1) * 36] = np.broadcast_to(rb[None, :], (128, 36))
    base = np.zeros((128, NCOL), np.float32)
    base[:, C_IDENT:C_IDENT + 128] = np.eye(128, dtype=np.float32)
    base[:, C_FNG:C_FNG + 8] = _col(f("final_norm_g"))
    base[:, C_INVC:C_INVC + 16] = np.broadcast_to((1.0 / np.arange(1, 17, dtype=np.float32))[None, :], (128, 16))
    for l in range(2):
        o = C_L0 + l * PL
        base[:, o + O_MNG:o + O_MNG + 8] = _col(f("mixer_norm_g")[l])
        base[:, o + O_BADA:o + O_BADA + 48] = _col(f("b_ada")[l])
        cw = f("conv_w")[l]
        for k in range(CONV_K):
            ck = _col(cw[k])
            for cc in range(4):
                base[:, o + O_CONVW + cc * CONV_K + k] = ck[:, cc]
        base[:, o + O_CONVB:o + O_CONVB + 4] = _col(f("conv_b")[l])
        base[:, o + O_LNG:o + O_LNG + 4] = _col(f("conv_ln_g")[l])
        base[:, o + O_LNB:o + O_LNB + 4] = _col(f("conv_ln_b")[l])
        base[:, o + O_BCO:o + O_BCO + 8] = _col(f("b_conv_out")[l])
        base[:, o + O_PSC:o + O_PSC + 8] = _col(f("pool_scale")[l])
        base[:, o + O_FNG2:o + O_FNG2 + 8] = _col(f("ffn_norm_g")[l])
    in_maps = []
    for b in range(B):
        cp = base.copy()
        cp[:, C_C:C_C + 8] = _col(f("c")[b])
        m = dict(shared)
        m["colp"] = cp
        m["rowp"] = rowp
        m["xT"] = np.ascontiguousarray(f("x")[b, :S, :].T)
        in_maps.append(m)
    return in_maps


_NC_CACHE = {}


def _get_nc(S, NL, final_norm, first_layer):
    key = (S, NL, final_norm, first_layer)
    if key not in _NC_CACHE:
        _NC_CACHE[key] = build_nc(S, NL, final_norm, first_layer)
    return _NC_CACHE[key]


def run(inputs, S=4096, NL=2, final_norm=True, first_layer=0, trace=False):
    in_maps = _prep(inputs, S)
    nc = _get_nc(S, NL, final_norm, first_layer)
    res = run_bass_kernel_spmd(nc, in_maps, core_ids=list(range(len(in_maps))), trace=trace)
    out = np.stack([np.ascontiguousarray(r["yT"].T) for r in res.results], axis=0)
    return out, res


def kernel(**inputs):
    out, _ = run(inputs, S=4096, NL=2, final_norm=True)
    return out.astype(np.float32)
```

```python
import contextlib
import numpy as np
import concourse.bass as bass
import concourse.mybir as mybir
from concourse.bass_utils import run_bass_kernel_spmd

F32 = mybir.dt.float32
BF16 = mybir.dt.bfloat16
AF = mybir.ActivationFunctionType
ALU = mybir.AluOpType
AX = mybir.AxisListType

D = 1024
NCH = 8
TT = 512
CONV_K = 31
HV = CONV_K - 1
HU = 15
POOL_W = (2, 4, 8, 16)
NE = 32
EPS = 1e-6

C_IDENT = 0
C_C = 128
C_FNG = 136
C_INVC = 144
C_L0 = 160
PL = 216
O_MNG, O_BADA, O_CONVW, O_CONVB, O_LNG, O_LNB, O_BCO, O_PSC, O_FNG2 = 0, 8, 56, 180, 184, 188, 192, 200, 208
NCOL = C_L0 + 2 * PL
NROW = 2 * 36

ENGS = ("pe", "act", "dve", "pool", "sp")


class Sched:
    def __init__(self, nc):
        self.nc = nc
        self.ops = {e: [] for e in ENGS}
        self.semval = {}
        self.seen = {e: {} for e in ENGS}
        self.buf = {}
        self.own = {e: "c_" + e for e in ENGS}
        for e in ENGS:
            self.semval[self.own[e]] = 0

    def _b(self, k):
        if k not in self.buf:
            self.buf[k] = [None, {}]
        return self.buf[k]

    def op(self, eng, fn, reads=(), writes=(), sem=None, inc=1):
        own = self.own[eng]
        deps = {}

        def add(s, v, kind):
            if s == own and sem is None and eng == "pe":
                return
            if deps.get(s, 0) < v:
                deps[s] = v

        for b in reads:
            w = self._b(b)[0]
            if w:
                add(w[0], w[1], "raw")
            if b.startswith("ps"):
                for s, v in self._b(b)[1].items():
                    if s != own:
                        add(s, v, "psrd")
        for b in writes:
            st = self._b(b)
            if st[0]:
                add(st[0][0], st[0][1], "waw")
            for s, v in st[1].items():
                add(s, v, "war")
        waits = []
        for s, v in deps.items():
            if self.seen[eng].get(s, 0) < v:
                self.seen[eng][s] = v
                waits.append((s, v))
        semname = sem or own
        if semname not in self.semval:
            self.semval[semname] = 0
        if inc:
            self.semval[semname] += inc
            val = self.semval[semname]
        else:
            val = self.semval[semname] + 1
        self.ops[eng].append((waits, fn, semname, inc))
        for b in reads:
            st = self._b(b)
            if st[1].get(semname, 0) < val:
                st[1][semname] = val
        for b in writes:
            st = self._b(b)
            st[0] = (semname, val)
            st[1] = {}
        return val

    def dma(self, eng, out, in_, sem, reads=(), writes=()):
        return self.op(eng, lambda e: e.dma_start(out=out, in_=in_), reads=reads, writes=writes, sem=sem, inc=16)

    def final_wait(self, eng, sems):
        waits = [(s, self.semval[s]) for s in sems if self.semval.get(s, 0) > 0]
        self.ops[eng].append((waits, None, None, 0))

    def emit(self):
        nc = self.nc
        with contextlib.ExitStack() as st:
            handles = {}
            for name in self.semval:
                handles[name] = st.enter_context(nc.semaphore(name))
            block = st.enter_context(nc.Block())

            def run(engobj, lst):
                for waits, fn, semname, inc in lst:
                    for s, v in waits:
                        engobj.wait_ge(handles[s], v)
                    if fn is None:
                        continue
                    ins = fn(engobj)
                    if inc:
                        ins.then_inc(handles[semname], inc)

            @block.tensor
            def _(e):
                run(e, self.ops["pe"])

            @block.scalar
            def _(e):
                run(e, self.ops["act"])

            @block.vector
            def _(e):
                run(e, self.ops["dve"])

            @block.gpsimd
            def _(e):
                run(e, self.ops["pool"])

            @block.sync
            def _(e):
                run(e, self.ops["sp"])


def build_nc(S=4096, NL=2, final_norm=True, first_layer=0):
    NT = S // TT
    nc = bass.Bass("TRN2", target_bir_lowering=False)

    def din(name, shape):
        return nc.dram_tensor(name, list(shape), F32, kind="ExternalInput").ap()

    xT = din("xT", [D, S])
    colp_d = din("colp", [128, NCOL])
    rowp_d = din("rowp", [128, NROW])
    w_ada = din("w_ada", [2, D, 6 * D])
    w_in = din("w_in", [2, D, 3584])
    w_co = din("w_co", [2, 512, D])
    w_pool = din("w_pool", [2, 512, 256])
    w_out = din("w_out", [2, D, D])
    w_r = din("w_r", [2, D, 36])
    w_g = din("w_g", [2, NE, D, 256])
    w_u = din("w_u", [2, NE, D, 256])
    w_d = din("w_d", [2, NE, 256, D])
    yT = nc.dram_tensor("yT", [D, S], F32, kind="ExternalOutput").ap()
    combT_d = nc.dram_tensor("combT_d", [32, TT], BF16, kind="Internal").ap()

    S_ = Sched(nc)
    st = contextlib.ExitStack()
    with st:
        def sb(name, shape, dt):
            return st.enter_context(nc.sbuf_tensor(name, list(shape), dt))

        colp = sb("colp_s", [128, NCOL], F32)
        rowp = sb("rowp_s", [128, NROW], F32)
        x = sb("x", [128, NCH, TT], F32)
        h = sb("h", [128, NCH, TT], BF16)
        mix = sb("mix", [128, NCH, TT], BF16)
        NTMP = 4
        tmp = [sb(f"tmp{i}", [128, TT], F32) for i in range(NTMP)]
        rstd = sb("rstd", [128, TT], F32)
        stat = [sb(f"stat{i}", [128, TT], F32) for i in range(3)]
        vb = sb("vb", [128, 4, HV + TT], BF16)
        vhalo = sb("vhalo", [128, 2, 4, HV], BF16)
        U = sb("U", [128, 4, HU + TT], F32)
        uhalo = sb("uhalo", [128, 2, 4, HU], F32)
        T1 = sb("T1", [128, HU + TT], F32)
        T2 = sb("T2", [128, HU + TT], F32)
        pooled = sb("pooled", [128, 4, TT], BF16)
        cvo = sb("cvo", [128, 4, TT], F32)
        cv = sb("cv", [128, 4, TT], BF16)
        NDG = 2
        dg = [sb(f"dg{i}", [128, CONV_K, 128], BF16) for i in range(NDG)]
        NWS = 4
        wsl = [sb(f"wsl{i}", [128, NCH, 512], BF16) for i in range(NWS)]
        wco = sb("wco", [128, 4, D], BF16)
        pw = sb("pw", [128, 4, 256], BF16)
        wr = sb("wr", [128, 2, NCH, 36], BF16)
        NGU = 3
        NED = 4
        egu = [sb(f"egu{i}", [128, NCH, 512], BF16) for i in range(NGU)]
        ed = [sb(f"ed{i}", [128, 2, D], BF16) for i in range(NED)]
        sgt = [sb(f"sgt{i}", [128, 2, TT], BF16) for i in range(2)]
        tbt = [sb(f"tbt{i}", [128, 2, TT], BF16) for i in range(2)]
        NACT = 4
        actt = [sb(f"actt{i}", [128, 2, TT], BF16) for i in range(NACT)]
        NCB = 3
        cbc = [sb(f"cbc{i}", [128, TT], BF16) for i in range(NCB)]
        combT = sb("combT", [32, TT], BF16)
        ident_bf = sb("ident_bf", [128, 128], BF16)
        ones_bf = sb("ones_bf", [128, 128], BF16)
        epsT = sb("epsT", [128, 1], F32)
        cact = sb("cact", [128, NCH], BF16)
        cwb = sb("cwb", [128, 2, 4 * CONV_K], BF16)
        mod = sb("mod", [128, 2, 48], F32)
        gs1 = sb("gs1", [128, 2, NCH], F32)
        gs2 = sb("gs2", [128, 2, NCH], F32)
        rl = sb("rl", [128, 4, 36], F32)
        gmask, d4, e4, den4 = [sb(f"r4_{i}", [128, 4, 4], F32) for i in range(4)]
        sel8, mask1, sel8b, mask2, cw8 = [sb(f"r8_{i}", [128, 4, 8], F32) for i in range(5)]
        gmax, ssum, pg, m1, m2, dd, w1, w2 = [sb(f"r1_{i}", [128, 4], F32) for i in range(8)]
        prod = sb("prod", [128, 4, 4, 8], F32)
        comb = sb("comb", [128, 4, 4, 8], F32)
        ps = [st.enter_context(nc.psum_tensor(f"ps{i}", [128, TT], F32)) for i in range(8)]

        ident = colp[:, C_IDENT:C_IDENT + 128]

        bank_ctr = [0]

        def bank():
            b = bank_ctr[0] % 8
            bank_ctr[0] += 1
            return b

        tmp_ctr = [0]

        def newtmp():
            i = tmp_ctr[0] % NTMP
            tmp_ctr[0] += 1
            return i

        def lcol(l, off, n=1):
            c = C_L0 + l * PL + off
            return colp[:, c:c + n]

        def mm_group(b, pairs, reads, M=128, N=TT, out=None):
            n = len(pairs)
            o = out if out is not None else ps[b][0:M, 0:N]
            for i, (lt, rh) in enumerate(pairs):
                S_.op("pe", (lambda e, lt=lt, rh=rh, i=i: e.matmul(o, lhsT=lt, rhs=rh, start=(i == 0), stop=(i == n - 1))),
                      reads=reads, writes=[f"ps{b}"], inc=1 if i == n - 1 else 0)

        wblocks = []
        for t_ in range(NT):
            for l_ in range(first_layer, first_layer + NL):
                wl_ = w_in[l_].rearrange("(kc p) n -> p kc n", p=128)
                wo_ = w_out[l_].rearrange("(kc p) n -> p kc n", p=128)
                for c0 in (0, 512, 1024, 1536, 2560, 2048, 3072):
                    wblocks.append(wl_[:, :, c0:c0 + 512])
                wblocks.append(wo_[:, :, 0:512])
                wblocks.append(wo_[:, :, 512:1024])
        ws_loaded = [0]

        def prefetch_to(k):
            while ws_loaded[0] < min(k, len(wblocks)):
                n = ws_loaded[0]
                i = n % NWS
                S_.dma("pool", wsl[i][:], wblocks[n], f"s_ws{i}", writes=[f"ws{i}"])
                ws_loaded[0] += 1

        mix_it = [0]

        S_.dma("sp", colp[:], colp_d, "s_colp", writes=["colp"])
        S_.dma("sp", rowp[:], rowp_d, "s_rowp", writes=["rowp"])
        S_.dma("pool", wr[:], w_r.rearrange("l (kc p) n -> p l kc n", p=128), "s_wr", writes=["wr"])
        S_.op("dve", lambda e: e.memset(ones_bf[:], 1.0), writes=["ones"])
        S_.op("dve", lambda e: e.memset(epsT[:], EPS), writes=["eps"])
        S_.op("dve", lambda e: e.tensor_copy(out=ident_bf[:], in_=ident), reads=["colp"], writes=["identbf"])
        for l_ in range(2):
            S_.op("dve", (lambda e, l_=l_: e.tensor_copy(out=cwb[:, l_, :], in_=lcol(l_, O_CONVW, 4 * CONV_K))), reads=["colp"], writes=["cwb"])
        S_.op("dve", lambda e: e.memset(vhalo[:], 0.0), writes=["vhalo"])
        S_.op("dve", lambda e: e.memset(uhalo[:], 0.0), writes=["uhalo"])
        S_.op("act", lambda e: e.activation(out=cact[:], in_=colp[:, C_C:C_C + NCH], func=AF.Silu), reads=["colp"], writes=["cact"])

        for l in range(first_layer, first_layer + NL):
            bm = bank()
            for j in range(24):
                sl = j % (2 * NGU)
                stage = egu[sl // 2][:, :, (sl % 2) * 256:(sl % 2 + 1) * 256]
                key = ("eg%d" if sl % 2 == 0 else "eu%d") % (sl // 2)
                S_.dma("pool", stage, w_ada[l, :, j * 256:(j + 1) * 256].rearrange("(kc p) n -> p kc n", p=128),
                       "s_" + key, writes=[key])
                for half in range(2):
                    cj = 2 * j + half
                    for kc in range(NCH):
                        S_.op("pe", (lambda e, stage=stage, half=half, kc=kc, cj=cj, bm=bm:
                                     e.matmul(ps[bm][:, cj:cj + 1], lhsT=stage[:, kc, half * 128:(half + 1) * 128],
                                              rhs=cact[:, kc:kc + 1], start=(kc == 0), stop=(kc == NCH - 1))),
                              reads=[key, "cact"], writes=[f"ps{bm}"], inc=1 if kc == NCH - 1 else 0)
            S_.op("dve", (lambda e, l=l, bm=bm: e.tensor_tensor(out=mod[:, l, :], in0=ps[bm][:, 0:48], in1=lcol(l, O_BADA, 48), op=ALU.add)),
                  reads=[f"ps{bm}", "colp"], writes=["mod"])
            S_.op("dve", (lambda e, l=l: e.scalar_tensor_tensor(out=gs1[:, l, :], in0=mod[:, l, 8:16], scalar=1.0, in1=lcol(l, O_MNG, 8),
                                                                 op0=ALU.add, op1=ALU.mult)),
                  reads=["mod", "colp"], writes=["gs1"])
            S_.op("dve", (lambda e, l=l: e.scalar_tensor_tensor(out=gs2[:, l, :], in0=mod[:, l, 32:40], scalar=1.0, in1=lcol(l, O_FNG2, 8),
                                                                 op0=ALU.add, op1=ALU.mult)),
                  reads=["mod", "colp"], writes=["gs2"])

        XK = [f"x{dc}" for dc in range(NCH)]
        HK = [f"h{dc}" for dc in range(NCH)]
        MK = [f"mix{dc}" for dc in range(NCH)]

        def rms_stats():
            for dc in range(NCH):
                S_.op("act", (lambda e, dc=dc: e.activation(out=mix[:, dc, :], in_=x[:, dc, :], func=AF.Square)),
                      reads=[XK[dc]], writes=[MK[dc]])
            b = bank()
            mm_group(b, [(ones_bf[:], mix[:, dc, :]) for dc in range(NCH)], reads=["ones"] + MK)
            S_.op("act", (lambda e, b=b: e.activation(out=rstd[:], in_=ps[b][:], func=AF.Sqrt, bias=epsT[:, 0:1], scale=1.0 / D)),
                  reads=[f"ps{b}", "eps"], writes=["rstd"])
            S_.op("dve", lambda e: e.reciprocal(out=rstd[:], in_=rstd[:]), reads=["rstd"], writes=["rstd"])

        def norm_to_h(gs_ap, sh_ap):
            rms_stats()
            for dc in range(NCH):
                ti = newtmp()
                S_.op("dve", (lambda e, dc=dc, ti=ti: e.tensor_tensor(out=tmp[ti][:], in0=x[:, dc, :], in1=rstd[:], op=ALU.mult)),
                      reads=[XK[dc], "rstd"], writes=[f"tmp{ti}"])
                S_.op("act", (lambda e, dc=dc, ti=ti: e.activation(out=h[:, dc, :], in_=tmp[ti][:], func=AF.Identity,
                                                                   bias=sh_ap[:, dc:dc + 1], scale=gs_ap[:, dc:dc + 1])),
                      reads=[f"tmp{ti}", "mod", "gs1", "gs2"], writes=[HK[dc]])

        dg_ctr = [0]

        def build_dg(l, cc):
            di = cc % NDG
            S_.op("dve", (lambda e, di=di, cc=cc: e.tensor_tensor(
                out=dg[di][:, :, :], in0=ident_bf[:].unsqueeze(1).broadcast_to([128, CONV_K, 128]),
                in1=cwb[:, l, cc * CONV_K:(cc + 1) * CONV_K].unsqueeze(2).broadcast_to([128, CONV_K, 128]), op=ALU.mult)),
                reads=["identbf", "cwb"], writes=[f"dg{di}"])

        def mixer(t, l):
            first = (t == 0)
            for cc in range(NDG):
                build_dg(l, cc)
            norm_to_h(gs1[:, l, :], mod[:, l, 0:8])
            base = 9 * mix_it[0]
            mix_it[0] += 1
            blk = lambda n: (base + n) % NWS
            import os as _os2
            _stop = int(_os2.environ.get("K_MIX_STOP", "9"))
            if _stop <= 1:
                return
            prefetch_to(base + 0 + NWS)
            S_.dma("pool", wco[:], w_co[l].rearrange("(cc p) n -> p cc n", p=128), "s_wco", writes=["wco"])
            S_.dma("pool", pw[:], w_pool[l].rearrange("(g p) n -> p g n", p=128), "s_pw", writes=["pw"])
            iA, iG, iU = blk(0), blk(1), blk(2)
            S_.op("act", (lambda e: e.activation(out=vb[:, :, 0:HV], in_=vhalo[:, l, :, :], func=AF.Copy)), reads=["vhalo"], writes=["vb"])
            for cc in range(4):
                ba = bank()
                mm_group(ba, [(wsl[iA][:, kc, cc * 128:(cc + 1) * 128], h[:, kc, :]) for kc in range(NCH)], reads=[f"ws{iA}"] + HK)
                bg = bank()
                mm_group(bg, [(wsl[iG][:, kc, cc * 128:(cc + 1) * 128], h[:, kc, :]) for kc in range(NCH)], reads=[f"ws{iG}"] + HK)
                ti = newtmp()
                S_.op("act", (lambda e, bg=bg, ti=ti: e.activation(out=tmp[ti][:], in_=ps[bg][:], func=AF.Sigmoid)),
                      reads=[f"ps{bg}"], writes=[f"tmp{ti}"])
                S_.op("dve", (lambda e, ba=ba, ti=ti, cc=cc: e.tensor_tensor(out=vb[:, cc, HV:HV + TT], in0=ps[ba][:], in1=tmp[ti][:], op=ALU.mult)),
                      reads=[f"ps{ba}", f"tmp{ti}"], writes=["vb"])
            S_.op("act", (lambda e: e.activation(out=vhalo[:, l, :, :], in_=vb[:, :, TT:TT + HV], func=AF.Copy)), reads=["vb"], writes=["vhalo"])
            if _stop <= 2:
                return
            prefetch_to(base + 2 + NWS)
            S_.op("act", (lambda e: e.activation(out=U[:, :, 0:HU], in_=uhalo[:, l, :, :], func=AF.Copy)), reads=["uhalo"], writes=["U"])
            for gi in range(4):
                b = bank()
                mm_group(b, [(wsl[iU][:, kc, gi * 128:(gi + 1) * 128], h[:, kc, :]) for kc in range(NCH)], reads=[f"ws{iU}"] + HK)
                S_.op("act", (lambda e, b=b, gi=gi: e.activation(out=U[:, gi, HU:HU + TT], in_=ps[b][:], func=AF.Copy)),
                      reads=[f"ps{b}"], writes=["U"])
            S_.op("act", (lambda e: e.activation(out=uhalo[:, l, :, :], in_=U[:, :, TT:TT + HU], func=AF.Copy)), reads=["U"], writes=["uhalo"])
            W_ = HU + TT
            for gi, w in enumerate(POOL_W):
                src, srck = U[:, gi, :], "U"
                lo = 0
                dsts = [(T1, "T1"), (T2, "T2")]
                for j in range(gi + 1):
                    sh = 1 << j
                    dst, dstk = dsts[j % 2]
                    S_.op("dve", (lambda e, dst=dst, src=src, lo=lo, sh=sh: e.tensor_tensor(
                        out=dst[:, lo + sh:W_], in0=src[:, lo + sh:W_], in1=src[:, lo:W_ - sh], op=ALU.add)),
                        reads=[srck], writes=[dstk])
                    src, srck = dst[:], dstk
                    lo += sh
                S_.op("dve", (lambda e, src=src, gi=gi, w=w: e.scalar_tensor_tensor(
                    out=pooled[:, gi, :], in0=src[:, HU:W_], scalar=1.0 / w, in1=U[:, gi, HU:W_], op0=ALU.mult, op1=ALU.subtract)),
                    reads=[srck, "U"], writes=["pooled"])
                if first:
                    n = w - 1
                    fxb, fxk = dsts[(gi + 1) % 2]
                    S_.op("dve", (lambda e, src=src, n=n, fxb=fxb: e.tensor_tensor(out=fxb[:, 0:n], in0=src[:, HU:HU + n],
                                                                                   in1=colp[:, C_INVC:C_INVC + n], op=ALU.mult)),
                          reads=[srck, "colp"], writes=[fxk])
                    S_.op("dve", (lambda e, fxb=fxb, n=n, gi=gi: e.tensor_tensor(out=pooled[:, gi, 0:n], in0=fxb[:, 0:n], in1=U[:, gi, HU:HU + n], op=ALU.subtract)),
                          reads=[fxk, "U"], writes=["pooled"])
            if _stop <= 3:
                return
            bs1 = bank()
            bs2 = bank()
            for cc in range(4):
                di = cc % NDG
                b = bank()
                mm_group(b, [(dg[di][:, k, :], vb[:, cc, k:k + TT]) for k in range(CONV_K)], reads=[f"dg{di}", "vb"])
                if cc + NDG < 4:
                    build_dg(l, cc + NDG)
                S_.op("act", (lambda e, b=b, cc=cc: e.activation(out=cvo[:, cc, :], in_=ps[b][:], func=AF.Identity, bias=lcol(l, O_CONVB + cc), scale=1.0)),
                      reads=[f"ps{b}", "colp"], writes=[f"cvo{cc}"])
                S_.op("dve", (lambda e, cc=cc: e.tensor_copy(out=mix[:, cc, :], in_=cvo[:, cc, :])), reads=[f"cvo{cc}"], writes=[MK[cc]])
                S_.op("act", (lambda e, cc=cc: e.activation(out=mix[:, 4 + cc, :], in_=cvo[:, cc, :], func=AF.Square)),
                      reads=[f"cvo{cc}"], writes=[MK[4 + cc]])
            mm_group(bs1, [(ones_bf[:], mix[:, cc, :]) for cc in range(4)], reads=["ones"] + MK[0:4])
            mm_group(bs2, [(ones_bf[:], mix[:, 4 + cc, :]) for cc in range(4)], reads=["ones"] + MK[4:8])
            preload_experts(l)
            GT = []
            for j in range(2):
                for hc in range(2):
                    GT.append((sgt[j][:, hc, :], f"sgt{j}_{hc}"))
            for j in range(2):
                for hc in range(2):
                    GT.append((tbt[j][:, hc, :], f"tbt{j}_{hc}"))
            for a in range(NACT):
                for hc in range(2):
                    GT.append((actt[a][:, hc, :], f"actt{a}_{hc}"))

            def gates(dc):
                half, q = dc // 4, dc % 4
                if q == 0:
                    prefetch_to(base + 3 + 2 * half + NWS)
                iGA, iGB = blk(3 + 2 * half), blk(4 + 2 * half)
                for (iw, off) in ((iGA, 0), (iGB, 8)):
                    bgx = bank()
                    mm_group(bgx, [(wsl[iw][:, kc, q * 128:(q + 1) * 128], h[:, kc, :]) for kc in range(NCH)], reads=[f"ws{iw}"] + HK)
                    gt, gk = GT[off + dc]
                    S_.op("act", (lambda e, bgx=bgx, gt=gt: e.activation(out=gt, in_=ps[bgx][:], func=AF.Sigmoid)), reads=[f"ps{bgx}"], writes=[gk])

            def ln_out(cc):
                S_.op("dve", (lambda e, cc=cc: e.tensor_tensor(out=cvo[:, cc, :], in0=cvo[:, cc, :], in1=stat[0][:], op=ALU.subtract)),
                      reads=[f"cvo{cc}", "stat0"], writes=[f"cvo{cc}"])
                S_.op("dve", (lambda e, cc=cc: e.tensor_tensor(out=cvo[:, cc, :], in0=cvo[:, cc, :], in1=stat[2][:], op=ALU.mult)),
                      reads=[f"cvo{cc}", "stat2"], writes=[f"cvo{cc}"])
                ti = newtmp()
                S_.op("act", (lambda e, cc=cc, ti=ti: e.activation(out=tmp[ti][:], in_=cvo[:, cc, :], func=AF.Sigmoid, bias=lcol(l, O_LNB + cc), scale=lcol(l, O_LNG + cc))),
                      reads=[f"cvo{cc}", "colp"], writes=[f"tmp{ti}"])
                S_.op("act", (lambda e, cc=cc: e.activation(out=cvo[:, cc, :], in_=cvo[:, cc, :], func=AF.Identity, bias=lcol(l, O_LNB + cc), scale=lcol(l, O_LNG + cc))),
                      reads=[f"cvo{cc}", "colp"], writes=[f"cvo{cc}"])
                S_.op("dve", (lambda e, cc=cc, ti=ti: e.tensor_tensor(out=cv[:, cc, :], in0=cvo[:, cc, :], in1=tmp[ti][:], op=ALU.mult)),
                      reads=[f"cvo{cc}", f"tmp{ti}"], writes=[f"cv{cc}"])

            S_.op("dve", (lambda e: e.tensor_scalar(out=stat[0][:], in0=ps[bs1][:], scalar1=1.0 / 512, scalar2=None, op0=ALU.mult)),
                  reads=[f"ps{bs1}"], writes=["stat0"])
            S_.op("dve", (lambda e: e.tensor_tensor(out=stat[1][:], in0=stat[0][:], in1=stat[0][:], op=ALU.mult)), reads=["stat0"], writes=["stat1"])
            S_.op("dve", (lambda e: e.scalar_tensor_tensor(out=stat[1][:], in0=ps[bs2][:], scalar=1.0 / 512, in1=stat[1][:], op0=ALU.mult, op1=ALU.subtract)),
                  reads=[f"ps{bs2}", "stat1"], writes=["stat1"])
            gates(0)
            S_.op("act", (lambda e: e.activation(out=stat[2][:], in_=stat[1][:], func=AF.Sqrt, bias=epsT[:, 0:1], scale=1.0)),
                  reads=["stat1", "eps"], writes=["stat2"])
            S_.op("dve", (lambda e: e.reciprocal(out=stat[2][:], in_=stat[2][:])), reads=["stat2"], writes=["stat2"])
            gates(1)
            ln_out(0)
            ln_out(1)
            gates(2)
            ln_out(2)
            ln_out(3)
            for dc in range(3, NCH):
                gates(dc)
            CVK = [f"cv{cc}" for cc in range(4)]
            for dc in range(NCH):
                bya = bank()
                mm_group(bya, [(wco[:, cc, dc * 128:(dc + 1) * 128], cv[:, cc, :]) for cc in range(4)], reads=["wco"] + CVK)
                byb = bank()
                g = dc // 2
                mm_group(byb, [(pw[:, g, (dc % 2) * 128:(dc % 2 + 1) * 128], pooled[:, g, :])], reads=["pw", "pooled"])
                (sa, sak), (sb_, sbk) = GT[dc], GT[8 + dc]
                ta, tb_ = newtmp(), newtmp()
                S_.op("dve", (lambda e, bya=bya, ta=ta, dc=dc, sa=sa: e.scalar_tensor_tensor(out=tmp[ta][:], in0=ps[bya][:], scalar=lcol(l, O_BCO + dc), in1=sa,
                                                                                          op0=ALU.add, op1=ALU.mult)),
                      reads=[f"ps{bya}", sak, "colp"], writes=[f"tmp{ta}"])
                S_.op("dve", (lambda e, byb=byb, tb_=tb_, dc=dc, sb_=sb_: e.scalar_tensor_tensor(out=tmp[tb_][:], in0=ps[byb][:], scalar=lcol(l, O_PSC + dc), in1=sb_,
                                                                                              op0=ALU.mult, op1=ALU.mult)),
                      reads=[f"ps{byb}", sbk, "colp"], writes=[f"tmp{tb_}"])
                S_.op("dve", (lambda e, ta=ta, tb_=tb_, dc=dc: e.tensor_tensor(out=mix[:, dc, :], in0=tmp[ta][:], in1=tmp[tb_][:], op=ALU.add)),
                      reads=[f"tmp{ta}", f"tmp{tb_}"], writes=[MK[dc]])
            if _stop <= 5:
                return
            for half in range(2):
                prefetch_to(base + 7 + half + NWS)
                iO = blk(7 + half)
                for q in range(4):
                    dc = half * 4 + q
                    b = bank()
                    mm_group(b, [(wsl[iO][:, kc, q * 128:(q + 1) * 128], mix[:, kc, :]) for kc in range(NCH)], reads=[f"ws{iO}"] + MK)
                    S_.op("dve", (lambda e, b=b, dc=dc: e.scalar_tensor_tensor(out=x[:, dc, :], in0=ps[b][:], scalar=mod[:, l, 16 + dc:17 + dc], in1=x[:, dc, :],
                                                                               op0=ALU.mult, op1=ALU.add)),
                          reads=[f"ps{b}", XK[dc], "mod"], writes=[XK[dc]])

        def load_egu(l, e_):
            i = e_ % NGU
            S_.dma("pool", egu[i][:, :, 0:256], w_g[l, e_].rearrange("(kc p) n -> p kc n", p=128), f"s_eg{i}", writes=[f"eg{i}"])
            S_.dma("pool", egu[i][:, :, 256:512], w_u[l, e_].rearrange("(kc p) n -> p kc n", p=128), f"s_eu{i}", writes=[f"eu{i}"])

        def load_ed(l, e_):
            i = e_ % NED
            S_.dma("pool", ed[i][:], w_d[l, e_].rearrange("(hc p) n -> p hc n", p=128), f"s_ed{i}", writes=[f"ed{i}"])

        def preload_experts(l):
            for e_ in range(NGU):
                load_egu(l, e_)
            for e_ in range(NED):
                load_ed(l, e_)

        NB = TT // 128

        def router_logits(l):
            b = 4
            for blk in range(NB):
                n = NCH
                for kc in range(NCH):
                    S_.op("pe", (lambda e, blk=blk, kc=kc, b=b: e.matmul(ps[b][:, blk * 36:(blk + 1) * 36], lhsT=h[:, kc, blk * 128:(blk + 1) * 128],
                                                                      rhs=wr[:, l, kc, :], start=(kc == 0), stop=(kc == NCH - 1))),
                          reads=HK + ["wr"], writes=[f"ps{b}"], inc=1 if kc == NCH - 1 else 0)
            return b

        def router_topk(l, b):
            V = lambda fn, reads, writes: S_.op("dve", fn, reads=reads, writes=writes)
            bc = lambda ap, shape: ap.broadcast_to(shape)
            rl3 = rl[:, :, :]
            gl = rl[:, :, 0:4]
            el = rl[:, :, 4:36].rearrange("p b (g e) -> p b g e", e=8)
            V(lambda e: e.tensor_tensor(out=rl3, in0=ps[b][:, 0:NB * 36].rearrange("p (b n) -> p b n", n=36),
                                        in1=bc(rowp[:, l * 36:(l + 1) * 36].unsqueeze(1), [128, NB, 36]), op=ALU.add), [f"ps{b}", "rowp"], ["rl"])
            V(lambda e: e.tensor_reduce(out=gmax[:], in_=gl, axis=AX.X, op=ALU.max), ["rl"], ["gmax"])
            V(lambda e: e.tensor_tensor(out=gmask[:], in0=gl, in1=bc(gmax[:].unsqueeze(2), [128, NB, 4]), op=ALU.is_equal), ["rl", "gmax"], ["gmask"])
            V(lambda e: e.tensor_tensor(out=d4[:], in0=gl, in1=bc(gmax[:].unsqueeze(2), [128, NB, 4]), op=ALU.subtract), ["rl", "gmax"], ["d4"])
            S_.op("act", lambda e: e.activation(out=d4[:], in_=d4[:], func=AF.Tanh, scale=0.5), reads=["d4"], writes=["d4"])
            V(lambda e: e.tensor_scalar(out=den4[:], in0=d4[:], scalar1=-1.0, scalar2=1.0, op0=ALU.mult, op1=ALU.add), ["d4"], ["den4"])
            V(lambda e: e.reciprocal(out=den4[:], in_=den4[:]), ["den4"], ["den4"])
            V(lambda e: e.scalar_tensor_tensor(out=e4[:], in0=d4[:], scalar=1.0, in1=den4[:], op0=ALU.add, op1=ALU.mult), ["d4", "den4"], ["e4"])
            V(lambda e: e.tensor_reduce(out=ssum[:], in_=e4[:], axis=AX.X, op=ALU.add), ["e4"], ["ssum"])
            V(lambda e: e.reciprocal(out=pg[:], in_=ssum[:]), ["ssum"], ["pg"])
            V(lambda e: e.tensor_tensor(out=prod[:], in0=el, in1=bc(gmask[:].unsqueeze(3), [128, NB, 4, 8]), op=ALU.mult), ["rl", "gmask"], ["prod"])
            V(lambda e: e.tensor_reduce(out=sel8[:], in_=prod[:].rearrange("p b g e -> p b e g"), axis=AX.X, op=ALU.add), ["prod"], ["sel8"])
            V(lambda e: e.tensor_reduce(out=m1[:], in_=sel8[:], axis=AX.X, op=ALU.max), ["sel8"], ["m1"])
            V(lambda e: e.tensor_tensor(out=mask1[:], in0=sel8[:], in1=bc(m1[:].unsqueeze(2), [128, NB, 8]), op=ALU.is_equal), ["sel8", "m1"], ["mask1"])
            V(lambda e: e.scalar_tensor_tensor(out=sel8b[:], in0=mask1[:], scalar=-1e30, in1=sel8[:], op0=ALU.mult, op1=ALU.add), ["mask1", "sel8"], ["sel8b"])
            V(lambda e: e.tensor_reduce(out=m2[:], in_=sel8b[:], axis=AX.X, op=ALU.max), ["sel8b"], ["m2"])
            V(lambda e: e.tensor_tensor(out=mask2[:], in0=sel8b[:], in1=bc(m2[:].unsqueeze(2), [128, NB, 8]), op=ALU.is_equal), ["sel8b", "m2"], ["mask2"])
            V(lambda e: e.tensor_tensor(out=dd[:], in0=m1[:], in1=m2[:], op=ALU.subtract), ["m1", "m2"], ["dd"])
            S_.op("act", lambda e: e.activation(out=dd[:], in_=dd[:], func=AF.Tanh, scale=0.5), reads=["dd"], writes=["dd"])
            V(lambda e: e.tensor_scalar(out=w1[:], in0=dd[:], scalar1=1.0, scalar2=0.5, op0=ALU.add, op1=ALU.mult), ["dd"], ["w1"])
            V(lambda e: e.tensor_tensor(out=w1[:], in0=w1[:], in1=pg[:], op=ALU.mult), ["w1", "pg"], ["w1"])
            V(lambda e: e.tensor_tensor(out=w2[:], in0=pg[:], in1=w1[:], op=ALU.subtract), ["w1", "pg"], ["w2"])
            V(lambda e: e.tensor_tensor(out=mask1[:], in0=mask1[:], in1=bc(w1[:].unsqueeze(2), [128, NB, 8]), op=ALU.mult), ["mask1", "w1"], ["mask1"])
            V(lambda e: e.tensor_tensor(out=mask2[:], in0=mask2[:], in1=bc(w2[:].unsqueeze(2), [128, NB, 8]), op=ALU.mult), ["mask2", "w2"], ["mask2"])
            V(lambda e: e.tensor_tensor(out=cw8[:], in0=mask1[:], in1=mask2[:], op=ALU.add), ["mask1", "mask2"], ["cw8"])
            V(lambda e: e.tensor_tensor(out=comb[:], in0=bc(cw8[:].unsqueeze(2), [128, NB, 4, 8]), in1=bc(gmask[:].unsqueeze(3), [128, NB, 4, 8]), op=ALU.mult),
              ["cw8", "gmask"], ["comb"])
            bt = 5
            for blk in range(NB):
                S_.op("pe", (lambda e, blk=blk: e.transpose(ps[bt][0:32, blk * 128:(blk + 1) * 128], comb[:, blk, :, :].rearrange("p g e -> p (g e)"), ident)),
                      reads=["comb", "colp"], writes=[f"ps{bt}"])
            S_.op("act", lambda e: e.activation(out=combT[:], in_=ps[bt][0:32, :], func=AF.Copy), reads=[f"ps{bt}"], writes=["combT"])

        def moe(t, l):
            norm_to_h(gs2[:, l, :], mod[:, l, 24:32])
            BA, BB, BC = (0, 1), (2, 3), (4, 5, 6, 7)
            g2c = lambda dc: mod[:, l, 40 + dc:41 + dc]
            dctr = [0]

            def load_cbc(e_):
                j = e_ % NCB
                S_.dma("sp", cbc[j][:], combT_d[e_:e_ + 1, :].broadcast_to([128, TT]), f"s_cbc{j}", reads=["combT_d"], writes=[f"cbc{j}"])

            def gate_up_pe(e_):
                i = e_ % NGU
                if e_ + 2 < NE:
                    load_cbc(e_ + 2)
                for hc, (bg, bu) in enumerate((BA, BB)):
                    mm_group(bg, [(egu[i][:, kc, hc * 128:(hc + 1) * 128], h[:, kc, :]) for kc in range(NCH)], reads=[f"eg{i}"] + HK)
                    mm_group(bu, [(egu[i][:, kc, 256 + hc * 128:256 + (hc + 1) * 128], h[:, kc, :]) for kc in range(NCH)], reads=[f"eu{i}"] + HK)
                if e_ + NGU < NE:
                    load_egu(l, e_ + NGU)

            def gate_up_ev(e_):
                j = e_ % 2
                a = e_ % NACT
                c = e_ % NCB
                for hc, (bg, bu) in enumerate((BA, BB)):
                    S_.op("act", (lambda e, bg=bg, hc=hc, j=j: e.activation(out=sgt[j][:, hc, :], in_=ps[bg][:], func=AF.Silu)),
                          reads=[f"ps{bg}"], writes=[f"sgt{j}_{hc}"])
                    S_.op("dve", (lambda e, bu=bu, hc=hc, j=j, c=c: e.tensor_tensor(out=tbt[j][:, hc, :], in0=ps[bu][:], in1=cbc[c][:], op=ALU.mult)),
                          reads=[f"ps{bu}", f"cbc{c}"], writes=[f"tbt{j}_{hc}"])
                for hc in range(2):
                    S_.op("dve", (lambda e, hc=hc, j=j, a=a: e.tensor_tensor(out=actt[a][:, hc, :], in0=tbt[j][:, hc, :], in1=sgt[j][:, hc, :], op=ALU.mult)),
                          reads=[f"tbt{j}_{hc}", f"sgt{j}_{hc}"], writes=[f"actt{a}_{hc}"])

            def gate_up(e_):
                gate_up_pe(e_)
                gate_up_ev(e_)

            def down_pair(p):
                es = (2 * p, 2 * p + 1)
                for dc in range(NCH):
                    bd = BC[dctr[0] % len(BC)]
                    dctr[0] += 1
                    pairs, rd = [], []
                    for e_ in es:
                        for hc in range(2):
                            pairs.append((ed[e_ % NED][:, hc, dc * 128:(dc + 1) * 128], actt[e_ % NACT][:, hc, :]))
                        rd += [f"ed{e_ % NED}", f"actt{e_ % NACT}_0", f"actt{e_ % NACT}_1"]
                    mm_group(bd, pairs, reads=rd)
                    S_.op("dve", (lambda e, bd=bd, dc=dc: e.scalar_tensor_tensor(out=x[:, dc, :], in0=ps[bd][:], scalar=g2c(dc), in1=x[:, dc, :],
                                                                                 op0=ALU.mult, op1=ALU.add)),
                          reads=[f"ps{bd}", XK[dc], "mod"], writes=[XK[dc]])
                for e_ in es:
                    if e_ + NED < NE:
                        load_ed(l, e_ + NED)

            rb = router_logits(l)
            i0 = 0 % NGU
            for hc, (bg, bu) in enumerate((BA, BB)):
                mm_group(bg, [(egu[i0][:, kc, hc * 128:(hc + 1) * 128], h[:, kc, :]) for kc in range(NCH)], reads=[f"eg{i0}"] + HK)
                mm_group(bu, [(egu[i0][:, kc, 256 + hc * 128:256 + (hc + 1) * 128], h[:, kc, :]) for kc in range(NCH)], reads=[f"eu{i0}"] + HK)
            router_topk(l, rb)
            S_.dma("sp", combT_d, combT[:], "s_combd", reads=["combT"], writes=["combT_d"])
            load_cbc(0)
            load_cbc(1)
            load_cbc(2)
            load_egu(l, NGU)
            gate_up_ev(0)
            gate_up(1)
            for p in range(NE // 2):
                if 2 * p + 2 < NE:
                    gate_up(2 * p + 2)
                    gate_up(2 * p + 3)
                down_pair(p)

        xv = xT.rearrange("(dc p) s -> p dc s", p=128)
        yv = yT.rearrange("(dc p) s -> p dc s", p=128)
        for t in range(NT):
            S_.dma("sp", x[:], xv[:, :, t * TT:(t + 1) * TT], "s_x", writes=XK)
            for l in range(first_layer, first_layer + NL):
                import os as _os
                _ph = _os.environ.get("K_PHASES", "mixer,moe")
                if "mixer" in _ph:
                    mixer(t, l)
                if "moe" in _ph:
                    moe(t, l)
            if final_norm:
                rms_stats()
                for dc in range(NCH):
                    S_.op("dve", (lambda e, dc=dc: e.scalar_tensor_tensor(out=x[:, dc, :], in0=x[:, dc, :], scalar=colp[:, C_FNG + dc:C_FNG + dc + 1], in1=rstd[:],
                                                                          op0=ALU.mult, op1=ALU.mult)),
                          reads=[XK[dc], "rstd", "colp"], writes=[XK[dc]])
            S_.dma("sp", yv[:, :, t * TT:(t + 1) * TT], x[:], "s_y", reads=XK)
        S_.final_wait("sp", ["s_y"])
        print("[kernel] sbuf bytes remaining/partition:", nc.sbuf_bytes_remaining)
        S_.emit()
    return nc


def _col(v):
    return np.ascontiguousarray(np.asarray(v, np.float32).reshape(-1, 128).T)


def _prep(inputs, S):
    f = lambda k: np.asarray(inputs[k], np.float32)
    B = f("x").shape[0]
    shared = {
        "w_ada": f("w_ada"), "w_in": f("w_in"), "w_co": f("w_conv_out"),
        "w_pool": np.ascontiguousarray(f("pool_w").reshape(2, 512, 256)),
        "w_out": f("w_out"),
        "w_r": np.ascontiguousarray(np.concatenate([f("w_router_group"), f("w_router_expert")], axis=-1)),
        "w_g": f("w_expert_gate"), "w_u": f("w_expert_up"), "w_d": f("w_expert_down"),
    }
    rowp = np.zeros((128, NROW), np.float32)
    for l in range(2):
        rb = np.concatenate([f("b_router_group")[l], f("b_router_expert")[l]])
        rowp[:, l * 36:(l + 1) * 36] = np.broadcast_to(rb[None, :], (128, 36))
    base = np.zeros((128, NCOL), np.float32)
    base[:, C_IDENT:C_IDENT + 128] = np.eye(128, dtype=np.float32)
    base[:, C_FNG:C_FNG + 8] = _col(f("final_norm_g"))
    base[:, C_INVC:C_INVC + 16] = np.broadcast_to((1.0 / np.arange(1, 17, dtype=np.float32))[None, :], (128, 16))
    for l in range(2):
        o = C_L0 + l * PL
        base[:, o + O_MNG:o + O_MNG + 8] = _col(f("mixer_norm_g")[l])
        base[:, o + O_BADA:o + O_BADA + 48] = _col(f("b_ada")[l])
        cw = f("conv_w")[l]
        for k in range(CONV_K):
            ck = _col(cw[k])
            for cc in range(4):
                base[:, o + O_CONVW + cc * CONV_K + k] = ck[:, cc]
        base[:, o + O_CONVB:o + O_CONVB + 4] = _col(f("conv_b")[l])
        base[:, o + O_LNG:o + O_LNG + 4] = _col(f("conv_ln_g")[l])
        base[:, o + O_LNB:o + O_LNB + 4] = _col(f("conv_ln_b")[l])
        base[:, o + O_BCO:o + O_BCO + 8] = _col(f("b_conv_out")[l])
        base[:, o + O_PSC:o + O_PSC + 8] = _col(f("pool_scale")[l])
        base[:, o + O_FNG2:o + O_FNG2 + 8] = _col(f("ffn_norm_g")[l])
    in_maps = []
    for b in range(B):
        cp = base.copy()
        cp[:, C_C:C_C + 8] = _col(f("c")[b])
        m = dict(shared)
        m["colp"] = cp
        m["rowp"] = rowp
        m["xT"] = np.ascontiguousarray(f("x")[b, :S, :].T)
        in_maps.append(m)
    return in_maps


_NC_CACHE = {}


def _get_nc(S, NL, final_norm, first_layer):
    key = (S, NL, final_norm, first_layer)
    if key not in _NC_CACHE:
        _NC_CACHE[key] = build_nc(S, NL, final_norm, first_layer)
    return _NC_CACHE[key]


def run(inputs, S=4096, NL=2, final_norm=True, first_layer=0, trace=False):
    in_maps = _prep(inputs, S)
    nc = _get_nc(S, NL, final_norm, first_layer)
    res = run_bass_kernel_spmd(nc, in_maps, core_ids=list(range(len(in_maps))), trace=trace)
    out = np.stack([np.ascontiguousarray(r["yT"].T) for r in res.results], axis=0)
    return out, res


def kernel(**inputs):
    out, _ = run(inputs, S=4096, NL=2, final_norm=True)
    return out.astype(np.float32)
```
